# Optimizing a Trainium2 kernel written in Bass

```python
import math
import jax, jax.numpy as jnp
from jax import lax
import numpy as np

D_MODEL = 1024
BATCH = 8
SEQ = 4096
DEPTH = 1

EPS = 1e-6
SSM_EXPAND = 2
SSM_D_INNER = SSM_EXPAND * D_MODEL
SSM_HEADDIM = 64
SSM_HEADS = SSM_D_INNER // SSM_HEADDIM
SSM_GROUPS = 4
SSM_HEADS_PER_GROUP = SSM_HEADS // SSM_GROUPS
SSM_STATE = 128
SSM_CONV = 4
SSM_CHUNK = 128
SSM_CONV_DIM = SSM_D_INNER + 2 * SSM_GROUPS * SSM_STATE
RET_HEADS = 4
RET_QK_DIM = 256
RET_V_DIM = 512
RET_QK_WIDTH = RET_HEADS * RET_QK_DIM
RET_V_WIDTH = RET_HEADS * RET_V_DIM
RET_CHUNK = 128
ROPE_BASE = 10000.0
N_EXPERTS = 256
TOP_K = 8
N_ROUTE_GROUPS = 8
TOPK_ROUTE_GROUPS = 4
EXPERT_DIM = 256
SHARED_DIM = 256
ROUTED_SCALE = 2.5
MOE_BLOCK = 128
IN_WIDTHS = (SSM_D_INNER, SSM_CONV_DIM, SSM_HEADS, RET_QK_WIDTH, RET_QK_WIDTH,
             RET_V_WIDTH, RET_V_WIDTH, D_MODEL, D_MODEL)
IN_DIM = sum(IN_WIDTHS)

kernel_name = "hybrid_ssd_retention_moe_adaln"


def rms_norm(x, w):
    xf = x.astype(jnp.float32)
    y = xf * lax.rsqrt(jnp.mean(xf * xf, axis=-1, keepdims=True) + EPS)
    return (y * w.astype(jnp.float32)).astype(x.dtype)


def causal_depthwise_conv(u, w, b):
    k = w.shape[0]
    out = lax.conv_general_dilated(u, w[:, None, :].astype(u.dtype), window_strides=(1,),
                                   padding=[(k - 1, 0)],
                                   dimension_numbers=('NWC', 'WIO', 'NWC'),
                                   feature_group_count=u.shape[-1])
    return out + b


def to_chunks(t, chunk):
    b, s = t.shape[:2]
    return jnp.moveaxis(t.reshape(b, s // chunk, chunk, *t.shape[2:]), 1, 0)


def from_chunks(t):
    t = jnp.moveaxis(t, 0, 1)
    return t.reshape(t.shape[0], t.shape[1] * t.shape[2], *t.shape[3:])


def ssd_chunked(xh, dt, A, bm, cm):
    bsz = xh.shape[0]
    L = SSM_CHUNK
    xh, dt, bm, cm = (t.astype(jnp.float32) for t in (xh, dt, bm, cm))
    a = dt * A.astype(jnp.float32)
    causal = jnp.tril(jnp.ones((L, L), dtype=bool))

    def step(state, inp):
        xc, dtc, ac, bc, cc = inp
        acum = jnp.cumsum(ac, axis=1)
        acum_t = jnp.moveaxis(acum, 1, -1)
        seg = acum_t[..., :, None] - acum_t[..., None, :]
        decay = jnp.exp(jnp.where(causal, seg, -jnp.inf))
        cb = jnp.einsum('blgn,bsgn->bgls', cc, bc)
        w = cb[:, :, None] * decay * jnp.moveaxis(dtc, 1, -1)[..., None, :]
        y_intra = jnp.einsum('bghls,bsghp->blghp', w, xc)
        y_inter = jnp.einsum('blgn,bghpn->blghp', cc, state) * jnp.exp(acum)[..., None]
        to_end = jnp.exp(acum[:, -1:] - acum) * dtc
        new_state = (state * jnp.exp(acum[:, -1])[..., None, None]
                     + jnp.einsum('bsgn,bsgh,bsghp->bghpn', bc, to_end, xc))
        return new_state, y_intra + y_inter

    state0 = jnp.zeros((bsz, SSM_GROUPS, SSM_HEADS_PER_GROUP, SSM_HEADDIM, SSM_STATE), jnp.float32)
    xs = tuple(to_chunks(t, L) for t in (xh, dt, a, bm, cm))
    _, ys = lax.scan(step, state0, xs)
    return from_chunks(ys)


def rotary(t, positions):
    half = t.shape[-1] // 2
    inv = 1.0 / (ROPE_BASE ** (jnp.arange(half, dtype=jnp.float32) / half))
    ang = positions.astype(jnp.float32)[..., None] * inv
    cos, sin = jnp.cos(ang)[:, :, None], jnp.sin(ang)[:, :, None]
    t1, t2 = t[..., :half], t[..., half:]
    return jnp.concatenate([t1 * cos - t2 * sin, t1 * sin + t2 * cos], axis=-1)


def retention_chunked(q, k, v):
    bsz = q.shape[0]
    L = RET_CHUNK
    log_gamma = jnp.log1p(-(2.0 ** (-5.0 - jnp.arange(RET_HEADS, dtype=jnp.float32))))
    idx = jnp.arange(L, dtype=jnp.float32)
    causal = jnp.tril(jnp.ones((L, L), dtype=bool))
    rel = jnp.where(causal, idx[:, None] - idx[None, :], 0.0)
    intra_decay = jnp.where(causal, jnp.exp(rel[None] * log_gamma[:, None, None]), 0.0)
    q_decay = jnp.exp((idx + 1.0)[:, None] * log_gamma[None])
    k_decay = jnp.exp((L - 1.0 - idx)[:, None] * log_gamma[None])
    chunk_decay = jnp.exp(L * log_gamma)

    def step(state, inp):
        qc, kc, vc = inp
        s = jnp.einsum('blhd,bshd->bhls', qc, kc) * intra_decay
        y = (jnp.einsum('bhls,bshv->blhv', s, vc)
             + jnp.einsum('blhd,bhdv->blhv', qc * q_decay[:, :, None], state))
        new_state = (state * chunk_decay[:, None, None]
                     + jnp.einsum('bshd,bshv->bhdv', kc * k_decay[:, :, None], vc))
        return new_state, y

    state0 = jnp.zeros((bsz, RET_HEADS, RET_QK_DIM, RET_V_DIM), jnp.float32)
    xs = tuple(to_chunks(t.astype(jnp.float32), L) for t in (q, k, v))
    _, ys = lax.scan(step, state0, xs)
    return from_chunks(ys)


def hybrid_mixer(h, positions, w_in, conv_w, conv_b, dt_bias, a_log, d_skip, ssm_norm_w,
                 w_ssm_out, w_ret_out, w_out):
    bsz, s, _ = h.shape
    split_points = np.cumsum(IN_WIDTHS)[:-1].tolist()
    z, xbc, dt, q, k, v, g, gate_s, gate_r = jnp.split(h @ w_in, split_points, axis=-1)

    xbc = jax.nn.silu(causal_depthwise_conv(xbc, conv_w, conv_b))
    xs, bm, cm = jnp.split(xbc, [SSM_D_INNER, SSM_D_INNER + SSM_GROUPS * SSM_STATE], axis=-1)
    xs = xs.reshape(bsz, s, SSM_GROUPS, SSM_HEADS_PER_GROUP, SSM_HEADDIM)
    bm = bm.reshape(bsz, s, SSM_GROUPS, SSM_STATE)
    cm = cm.reshape(bsz, s, SSM_GROUPS, SSM_STATE)
    dt = jax.nn.softplus(dt.astype(jnp.float32) + dt_bias.astype(jnp.float32))
    dt = dt.reshape(bsz, s, SSM_GROUPS, SSM_HEADS_PER_GROUP)
    A = -jnp.exp(a_log.astype(jnp.float32)).reshape(SSM_GROUPS, SSM_HEADS_PER_GROUP)
    y = ssd_chunked(xs, dt, A, bm, cm) + xs.astype(jnp.float32) * d_skip.reshape(
        SSM_GROUPS, SSM_HEADS_PER_GROUP, 1).astype(jnp.float32)
    y = y.reshape(bsz, s, SSM_D_INNER) * jax.nn.silu(z.astype(jnp.float32))
    yg = y.reshape(bsz, s, SSM_GROUPS, SSM_D_INNER // SSM_GROUPS)
    yg = yg * lax.rsqrt(jnp.mean(yg * yg, axis=-1, keepdims=True) + EPS)
    y = (yg.reshape(bsz, s, SSM_D_INNER) * ssm_norm_w.astype(jnp.float32)).astype(h.dtype)
    y_ssm = y @ w_ssm_out

    q = rotary(q.reshape(bsz, s, RET_HEADS, RET_QK_DIM).astype(jnp.float32), positions)
    k = rotary(k.reshape(bsz, s, RET_HEADS, RET_QK_DIM).astype(jnp.float32), positions) * (RET_QK_DIM ** -0.5)
    v = v.reshape(bsz, s, RET_HEADS, RET_V_DIM)
    yr = retention_chunked(q, k, v)
    mu = jnp.mean(yr, axis=-1, keepdims=True)
    var = jnp.mean(jnp.square(yr - mu), axis=-1, keepdims=True)
    yr = ((yr - mu) * lax.rsqrt(var + EPS)).reshape(bsz, s, RET_V_WIDTH)
    yr = (jax.nn.silu(g.astype(jnp.float32)) * yr).astype(h.dtype)
    y_ret = yr @ w_ret_out

    merged = jax.nn.sigmoid(gate_s) * y_ssm + jax.nn.sigmoid(gate_r) * y_ret
    return merged @ w_out


def swiglu(x, w_gate, w_up, w_down):
    return (jax.nn.silu(x @ w_gate) * (x @ w_up)) @ w_down


def moe(h, w_router, router_bias, w_exp_gate, w_exp_up, w_exp_down, w_sh_gate, w_sh_up, w_sh_down):
    bsz, s, d = h.shape
    T = bsz * s
    xf = h.reshape(T, d)
    scores = jax.nn.sigmoid(xf.astype(jnp.float32) @ w_router.astype(jnp.float32))
    sel = scores + router_bias.astype(jnp.float32)
    grp = sel.reshape(T, N_ROUTE_GROUPS, N_EXPERTS // N_ROUTE_GROUPS)
    grp_score = jnp.sum(lax.top_k(grp, 2)[0], axis=-1)
    _, top_groups = lax.top_k(grp_score, TOPK_ROUTE_GROUPS)
    gmask = jnp.any(top_groups[..., None] == jnp.arange(N_ROUTE_GROUPS), axis=-2)
    gmask = jnp.repeat(gmask, N_EXPERTS // N_ROUTE_GROUPS, axis=-1)
    _, idx = lax.top_k(jnp.where(gmask, sel, -jnp.inf), TOP_K)
    wts = jnp.take_along_axis(scores, idx, axis=-1)
    wts = wts / (jnp.sum(wts, axis=-1, keepdims=True) + 1e-20) * ROUTED_SCALE

    n_assign = T * TOP_K
    flat_e = idx.reshape(-1).astype(jnp.int32)
    flat_tok = jnp.arange(n_assign, dtype=jnp.int32) // TOP_K
    flat_w = wts.reshape(-1)
    order = jnp.argsort(flat_e, stable=True)
    e_sorted, tok_sorted, w_sorted = flat_e[order], flat_tok[order], flat_w[order]
    counts = jnp.zeros((N_EXPERTS,), jnp.int32).at[flat_e].add(1)
    padded = (counts + MOE_BLOCK - 1) // MOE_BLOCK * MOE_BLOCK
    group_start = jnp.cumsum(counts) - counts
    padded_end = jnp.cumsum(padded)
    padded_start = padded_end - padded
    dest = padded_start[e_sorted] + (jnp.arange(n_assign, dtype=jnp.int32) - group_start[e_sorted])
    n_blocks = -(-n_assign // MOE_BLOCK) + N_EXPERTS
    slot_tok = jnp.full((n_blocks * MOE_BLOCK,), T, jnp.int32).at[dest].set(tok_sorted)
    slot_w = jnp.zeros((n_blocks * MOE_BLOCK,), jnp.float32).at[dest].set(w_sorted)
    block_expert = jnp.minimum(
        jnp.searchsorted(padded_end, jnp.arange(n_blocks, dtype=jnp.int32) * MOE_BLOCK, side='right'),
        N_EXPERTS - 1).astype(jnp.int32)
    xpad = jnp.concatenate([xf, jnp.zeros((1, d), xf.dtype)], axis=0)

    def block_step(acc, inp):
        toks, ws, e = inp
        yb = swiglu(xpad[toks], w_exp_gate[e], w_exp_up[e], w_exp_down[e])
        return acc.at[toks].add(yb.astype(jnp.float32) * ws[:, None]), None

    acc0 = jnp.zeros((T + 1, d), jnp.float32)
    routed, _ = lax.scan(block_step, acc0,
                         (slot_tok.reshape(n_blocks, MOE_BLOCK), slot_w.reshape(n_blocks, MOE_BLOCK), block_expert))
    shared = swiglu(xf, w_sh_gate, w_sh_up, w_sh_down).astype(jnp.float32)
    return (routed[:T] + shared).reshape(bsz, s, d).astype(h.dtype)


def setup_inputs(seed: int = 0) -> dict:
    key = jax.random.key(seed)
    ks = jax.random.split(key, 32)
    f32 = jnp.float32
    nrm = lambda k, shape, scale: jax.random.normal(k, shape, f32) * scale
    L_ = DEPTH
    dt0 = jnp.exp(jax.random.uniform(ks[9], (L_, SSM_HEADS), f32) * (math.log(0.1) - math.log(0.001)) + math.log(0.001))
    offs = jax.random.randint(ks[2], (BATCH, 1), 0, 1024, dtype=jnp.int32)
    return {
        "x": nrm(ks[0], (BATCH, SEQ, D_MODEL), 1.0),
        "c": nrm(ks[1], (BATCH, D_MODEL), 1.0),
        "positions": offs + jnp.arange(SEQ, dtype=jnp.int32)[None, :],
        "w_ada": nrm(ks[3], (L_, D_MODEL, 6 * D_MODEL), 0.5 * D_MODEL ** -0.5),
        "b_ada": nrm(ks[4], (L_, 6 * D_MODEL), 0.01),
        "norm1_w": 1.0 + nrm(ks[5], (L_, D_MODEL), 0.01),
        "w_in": nrm(ks[6], (L_, D_MODEL, IN_DIM), D_MODEL ** -0.5),
        "conv_w": nrm(ks[7], (L_, SSM_CONV, SSM_CONV_DIM), SSM_CONV ** -0.5),
        "conv_b": nrm(ks[8], (L_, SSM_CONV_DIM), 0.01),
        "dt_bias": dt0 + jnp.log(-jnp.expm1(-dt0)),
        "a_log": jnp.log(jax.random.uniform(ks[10], (L_, SSM_HEADS), f32, minval=1.0, maxval=16.0)),
        "d_skip": 1.0 + nrm(ks[11], (L_, SSM_HEADS), 0.01),
        "ssm_norm_w": 1.0 + nrm(ks[12], (L_, SSM_D_INNER), 0.01),
        "w_ssm_out": nrm(ks[13], (L_, SSM_D_INNER, D_MODEL), SSM_D_INNER ** -0.5),
        "w_ret_out": nrm(ks[14], (L_, RET_V_WIDTH, D_MODEL), RET_V_WIDTH ** -0.5),
        "w_out": nrm(ks[15], (L_, D_MODEL, D_MODEL), D_MODEL ** -0.5),
        "norm2_w": 1.0 + nrm(ks[16], (L_, D_MODEL), 0.01),
        "w_router": nrm(ks[17], (L_, D_MODEL, N_EXPERTS), D_MODEL ** -0.5),
        "router_bias": nrm(ks[18], (L_, N_EXPERTS), 0.01),
        "w_exp_gate": nrm(ks[19], (L_, N_EXPERTS, D_MODEL, EXPERT_DIM), D_MODEL ** -0.5),
        "w_exp_up": nrm(ks[20], (L_, N_EXPERTS, D_MODEL, EXPERT_DIM), D_MODEL ** -0.5),
        "w_exp_down": nrm(ks[21], (L_, N_EXPERTS, EXPERT_DIM, D_MODEL), EXPERT_DIM ** -0.5),
        "w_sh_gate": nrm(ks[22], (L_, D_MODEL, SHARED_DIM), D_MODEL ** -0.5),
        "w_sh_up": nrm(ks[23], (L_, D_MODEL, SHARED_DIM), D_MODEL ** -0.5),
        "w_sh_down": nrm(ks[24], (L_, SHARED_DIM, D_MODEL), SHARED_DIM ** -0.5),
        "final_norm_w": 1.0 + nrm(ks[25], (D_MODEL,), 0.01),
    }


def reference(x, c, positions, w_ada, b_ada, norm1_w, w_in, conv_w, conv_b, dt_bias, a_log, d_skip,
              ssm_norm_w, w_ssm_out, w_ret_out, w_out, norm2_w, w_router, router_bias,
              w_exp_gate, w_exp_up, w_exp_down, w_sh_gate, w_sh_up, w_sh_down, final_norm_w):
    for l in range(DEPTH):
        mod = jax.nn.silu(c) @ w_ada[l] + b_ada[l]
        shift1, scale1, gate1, shift2, scale2, gate2 = (m[:, None, :] for m in jnp.split(mod, 6, axis=-1))
        h = rms_norm(x, norm1_w[l]) * (1.0 + scale1) + shift1
        x = x + gate1 * hybrid_mixer(h, positions, w_in[l], conv_w[l], conv_b[l], dt_bias[l], a_log[l],
                                     d_skip[l], ssm_norm_w[l], w_ssm_out[l], w_ret_out[l], w_out[l])
        h = rms_norm(x, norm2_w[l]) * (1.0 + scale2) + shift2
        x = x + gate2 * moe(h, w_router[l], router_bias[l], w_exp_gate[l], w_exp_up[l], w_exp_down[l],
                            w_sh_gate[l], w_sh_up[l], w_sh_down[l])
    return rms_norm(x, final_norm_w)
```

```python
import numpy as np
from contextlib import ExitStack
from concourse.bass_utils import run_bass_kernel_spmd
import concourse.bass as bass
import concourse.mybir as mybir

F32 = mybir.dt.float32
BF16 = mybir.dt.bfloat16
I32 = mybir.dt.int32
U32 = mybir.dt.uint32
AF = mybir.ActivationFunctionType
ALU = mybir.AluOpType
AX = mybir.AxisListType


class _Op:
    __slots__ = ("eng", "fn", "reads", "writes", "is_dma", "deps", "sem", "val", "needs_inc", "idx", "pe_acc")

    def __init__(self, eng, fn, reads, writes, is_dma):
        self.eng = eng
        self.fn = fn
        self.reads = reads
        self.writes = writes
        self.is_dma = is_dma
        self.deps = []
        self.sem = None
        self.val = 0
        self.needs_inc = False


class Prog:
    ENGS = ("pe", "act", "dve", "pool", "sp")
    NDMA_SEM = {"sp": 6, "act": 2, "pool": 6}

    def __init__(self, nc):
        self.nc = nc
        self.ops = []
        self.last_writer = {}
        self.readers = {}
        self.alias = {}

    def _add(self, eng, fn, reads, writes, is_dma):
        reads = tuple(self.alias.get(k, k) if isinstance(k, str) else k for k in reads)
        writes = tuple(self.alias.get(k, k) if isinstance(k, str) else k for k in writes)
        op = _Op(eng, fn, reads, writes, is_dma)
        op.idx = len(self.ops)
        deps = set()
        for r in op.reads:
            w = self.last_writer.get(r)
            if w is not None:
                deps.add(w)
        for w_ in op.writes:
            w = self.last_writer.get(w_)
            if w is not None:
                deps.add(w)
            for rd in self.readers.get(w_, ()):
                deps.add(rd)
        deps.discard(op.idx)
        op.deps = sorted(deps)
        for r in op.reads:
            self.readers.setdefault(r, []).append(op.idx)
        for w_ in op.writes:
            self.last_writer[w_] = op.idx
            self.readers[w_] = []
        self.ops.append(op)
        return op

    def op(self, eng, fn, reads=(), writes=()):
        return self._add(eng, fn, reads, writes, False)

    def dma(self, queue, fn, reads=(), writes=()):
        return self._add(queue, fn, reads, writes, True)

    def barrier(self, touch):
        keys = list(set(self.last_writer.keys()) | set(self.readers.keys()))
        for e, (is_dma, fn) in touch.items():
            self._add(e, fn, (), keys + ["__bar"], is_dma)
        for e, (is_dma, fn) in touch.items():
            self._add(e, fn, ["__bar"], [("__bar2", e)], is_dma)

    def emit(self, stack):
        nc = self.nc
        ops = self.ops
        eng_sem = {}
        for e in ("pe", "act", "dve", "pool"):
            eng_sem[e] = stack.enter_context(nc.semaphore("s_" + e))
        dma_sems = {}
        for q, n in self.NDMA_SEM.items():
            dma_sems[q] = [stack.enter_context(nc.semaphore("d_%s%d" % (q, i))) for i in range(n)]
        dma_count = {q: 0 for q in self.NDMA_SEM}
        sem_last = {}
        sem_cnt = {}
        for o in ops:
            if o.is_dma:
                q = o.eng
                i = dma_count[q]
                dma_count[q] += 1
                s = dma_sems[q][i % len(dma_sems[q])]
                key = (q, i % len(dma_sems[q]))
                prev = sem_last.get(key)
                if prev is not None and prev not in o.deps:
                    o.deps.append(prev)
                sem_last[key] = o.idx
                sem_cnt[key] = sem_cnt.get(key, 0) + 1
                o.sem = s
                o.val = 16 * sem_cnt[key]
                o.needs_inc = True
        for o in ops:
            for d in o.deps:
                p = ops[d]
                if p.is_dma:
                    continue
                if p.eng == "pe" and o.eng == "pe" and not o.is_dma:
                    continue
                p.needs_inc = True
        cnt = {e: 0 for e in eng_sem}
        for o in ops:
            if not o.is_dma:
                o.sem = eng_sem[o.eng]
                if o.needs_inc:
                    cnt[o.eng] += 1
                o.val = cnt[o.eng]
        streams = {e: [] for e in self.ENGS}
        for o in ops:
            streams[o.eng].append(o)
        self.n_wait = 0

        def run(engname, eng):
            waited = {}
            for o in streams[engname]:
                need = {}
                for d in o.deps:
                    p = ops[d]
                    if (not p.is_dma) and p.eng == "pe" and engname == "pe" and not o.is_dma:
                        continue
                    k = id(p.sem)
                    if waited.get(k, 0) >= p.val:
                        continue
                    if k not in need or need[k][1] < p.val:
                        need[k] = (p.sem, p.val)
                for k, (s, v) in need.items():
                    eng.wait_ge(s, v)
                    waited[k] = v
                    self.n_wait += 1
                ins = o.fn(eng)
                if o.needs_inc:
                    ins.then_inc(o.sem, 16 if o.is_dma else 1)
            if engname in dma_sems:
                for i, s in enumerate(dma_sems[engname]):
                    key = (engname, i)
                    if key in sem_cnt and waited.get(id(s), 0) < 16 * sem_cnt[key]:
                        eng.wait_ge(s, 16 * sem_cnt[key])

        block = stack.enter_context(nc.Block())

        @block.tensor
        def _(e):
            run("pe", e)

        @block.scalar
        def _(e):
            run("act", e)

        @block.vector
        def _(e):
            run("dve", e)

        @block.gpsimd
        def _(e):
            run("pool", e)

        @block.sync
        def _(e):
            run("sp", e)


import math

D = 1024
NH_S = 32
HP = 64
NST = 128
NG = 4
IN_DIM = 13344
OFF_Z, OFF_XBC, OFF_DT, OFF_Q, OFF_K, OFF_V, OFF_G, OFF_GS, OFF_GR = 0, 2048, 5120, 5152, 6176, 7200, 9248, 11296, 12320
EPS = 1e-6
NE = 256


class Arena:
    def __init__(self, t, nbytes):
        self.t = t
        self.nbytes = nbytes
        self.off = 0

    def alloc(self, free_shape, dt):
        esz = 2 if dt == BF16 else 4
        n = 1
        for s in free_shape:
            n *= s
        nb = n * esz
        self.off = (self.off + 63) // 64 * 64
        assert self.off + nb <= self.nbytes, ("arena overflow", self.off, nb)
        v = self.t[:, self.off // 2:(self.off + nb) // 2]
        self.off += nb
        if dt != BF16:
            v = v.bitcast(dt)
        if len(free_shape) == 2:
            v = v.rearrange("p (a b) -> p a b", a=free_shape[0])
        elif len(free_shape) == 3:
            v = v.rearrange("p (a b c) -> p a b c", a=free_shape[0], b=free_shape[1])
        return v


def build(nc, S, dbg=False, stop=99):
    NCH = S // 128
    st = ExitStack()
    P = Prog(nc)
    dram = lambda name, shape, dt, kind="ExternalInput": nc.dram_tensor(name, list(shape), dt, kind=kind).ap()
    x_d = dram("x", [S, D], F32)
    pos_d = dram("pos", [1, S], I32)
    ccol_d = dram("ccol", [128, 8], F32)
    wada_d = dram("w_ada", [D, 6 * D], F32)
    bada_d = dram("b_ada", [1, 6 * D], F32)
    n1_d = dram("n1col", [128, 8], F32)
    n2_d = dram("n2col", [128, 8], F32)
    win_d = dram("w_in", [D, IN_DIM], F32)
    convw_d = dram("convw", [128, 24, 4], F32)
    convb_d = dram("convb", [128, 24], F32)
    dtb_d = dram("dt_bias", [1, 32], F32)
    alog_d = dram("a_log", [1, 32], F32)
    dsk_d = dram("d_skip", [1, 32], F32)
    snw_d = dram("ssm_norm_w", [1, 2048], F32)
    wso_d = dram("w_ssm_out", [2048, D], F32)
    wro_d = dram("w_ret_out", [2048, D], F32)
    wo_d = dram("w_out", [D, D], F32)
    wr_d = dram("w_router", [D, NE], F32)
    rb_d = dram("router_bias", [1, NE], F32)
    wsg_d = dram("w_sh_gate", [D, 256], F32)
    wsu_d = dram("w_sh_up", [D, 256], F32)
    wsd_d = dram("w_sh_down", [256, D], F32)
    fnw_d = dram("final_norm_w", [1, D], F32)
    out_d = dram("out", [S, D], F32, kind="ExternalOutput")
    winb_d = dram("winb", [D, IN_DIM], BF16, kind="Internal")
    wsob_d = dram("wsob", [2048, D], BF16, kind="Internal")
    wrob_d = dram("wrob", [2048, D], BF16, kind="Internal")
    wob_d = dram("wob", [D, D], BF16, kind="Internal")
    x2_d = dram("x2s", [S, D], F32, kind="Internal")
    h2T_d = dram("h2Ts", [128, 8, S], BF16, kind="Internal")
    weL_d = dram("weL", [NE * 128, 6144], F32)
    NB = S * 8 // 128 + NE
    h2tok_d = dram("h2tok", [S + 16, D], BF16, kind="Internal")
    tab_d = dram("tab", [128 * NB, 2], I32, kind="Internal")
    yall_d = dram("yall", [S * 8 + 128, D], F32, kind="Internal")
    dbg_d = {}

    ARB = 206 * 1024
    big = st.enter_context(nc.sbuf_tensor("big", [128, ARB // 2], BF16))
    A = Arena(big, ARB)
    psf = [st.enter_context(nc.psum_tensor("psf%d" % i, [128, 512], F32)) for i in range(6)]
    psb = [st.enter_context(nc.psum_tensor("psb%d" % i, [128, 1024], BF16)) for i in range(2)]

    V = lambda eng, fn, r, w: P.op(eng, fn, reads=r, writes=w)
    DMA = lambda q, fn, r, w: P.dma(q, fn, reads=r, writes=w)

    def mm(out, lhsT, rhs, start, stop, r, w):
        P.op("pe", lambda e: e.matmul(out, lhsT=lhsT, rhs=rhs, start=start, stop=stop), reads=r, writes=w)

    def tr(out, in_, ident, r, w):
        P.op("pe", lambda e: e.transpose(out, in_, ident), reads=r, writes=w)

    idi = A.alloc([128], I32)
    idf = A.alloc([128], F32)
    ident_b = A.alloc([128], BF16)
    ident_f = A.alloc([128], F32)
    tri_f = A.alloc([128], F32)
    ones_f = A.alloc([128], F32)
    negm8 = A.alloc([8, 128], BF16)
    idecT = A.alloc([4, 128], F32)
    qdec_bc = A.alloc([4, 128], F32)
    kdec = A.alloc([4], F32)
    pcol_i = A.alloc([1], I32)
    pcol = A.alloc([1], F32)
    invf = A.alloc([1], F32)
    V("pool", lambda e: e.iota(idi, pattern=[[1, 128]], base=0, channel_multiplier=-1), [], ["idi"])
    V("dve", lambda e: e.tensor_copy(idf, idi), ["idi"], ["idf"])
    V("dve", lambda e: e.tensor_single_scalar(ident_b, idf, 0.0, op=ALU.is_equal), ["idf"], ["ident_b"])
    V("dve", lambda e: e.tensor_single_scalar(ident_f, idf, 0.0, op=ALU.is_equal), ["idf"], ["ident_f"])
    V("dve", lambda e: e.tensor_single_scalar(tri_f, idf, 0.0, op=ALU.is_ge), ["idf"], ["tri_f"])
    V("dve", lambda e: e.memset(ones_f, 1.0), [], ["ones_f"])
    for i in range(8):
        V("dve", (lambda i: lambda e: e.tensor_scalar(out=negm8[:, i, :], in0=idf, scalar1=0.0, scalar2=-30000.0, op0=ALU.is_lt, op1=ALU.mult))(i), ["idf"], ["negm8"])
    V("pool", lambda e: e.iota(pcol_i, pattern=[[0, 1]], base=0, channel_multiplier=1), [], ["pcol_i"])
    V("dve", lambda e: e.tensor_copy(pcol, pcol_i), ["pcol_i"], ["pcol"])
    V("act", lambda e: e.activation(out=invf, in_=pcol, func=AF.Exp, scale=-math.log(10000.0) / 128.0), ["pcol"], ["invf"])
    lg = [math.log1p(-(2.0 ** (-5.0 - h))) for h in range(4)]
    tmpc = A.alloc([128], F32)
    for h in range(4):
        V("dve", lambda e: e.tensor_scalar(out=tmpc, in0=idf, scalar1=0.0, scalar2=None, op0=ALU.max), ["idf", "tmpc"], ["tmpc"])
        V("act", (lambda h: lambda e: e.activation(out=idecT[:, h, :], in_=tmpc, func=AF.Exp, scale=lg[h]))(h), ["tmpc"], ["idecT"])
        V("dve", (lambda h: lambda e: e.tensor_tensor(out=idecT[:, h, :], in0=idecT[:, h, :], in1=tri_f, op=ALU.mult))(h), ["idecT", "tri_f"], ["idecT"])
        V("dve", lambda e: e.tensor_scalar(out=tmpc, in0=idf, scalar1=pcol[:, 0:1], scalar2=1.0, op0=ALU.add, op1=ALU.add), ["idf", "pcol", "tmpc"], ["tmpc"])
        V("act", (lambda h: lambda e: e.activation(out=qdec_bc[:, h, :], in_=tmpc, func=AF.Exp, scale=lg[h]))(h), ["tmpc"], ["qdec_bc"])
        V("dve", lambda e: e.tensor_scalar(out=tmpc[:, 0:1], in0=pcol, scalar1=-1.0, scalar2=127.0, op0=ALU.mult, op1=ALU.add), ["pcol", "tmpc"], ["tmpc"])
        V("act", (lambda h: lambda e: e.activation(out=kdec[:, h:h + 1], in_=tmpc[:, 0:1], func=AF.Exp, scale=lg[h]))(h), ["tmpc"], ["kdec"])
    cdec = [math.exp(128.0 * lg[h]) for h in range(4)]

    ccol = A.alloc([8], F32)
    n1col = A.alloc([8], F32)
    n2col = A.alloc([8], F32)
    convw = A.alloc([24, 4], F32)
    convb = A.alloc([24], F32)
    dtb_bc = A.alloc([32], F32)
    A_bc = A.alloc([32], F32)
    D_bc = A.alloc([32], F32)
    snw_bc = A.alloc([2048], F32)
    rb_bc = A.alloc([NE], F32)
    for dst, src, k in ((ccol, ccol_d, "ccol"), (n1col, n1_d, "n1col"), (n2col, n2_d, "n2col"), (convw, convw_d, "convw"), (convb, convb_d, "convb")):
        DMA("sp", (lambda dst, src: lambda e: e.dma_start(out=dst, in_=src))(dst, src), [], [k])
    for dst, src, k, n in ((dtb_bc, dtb_d, "dtb_bc", 32), (A_bc, alog_d, "A_bc", 32), (D_bc, dsk_d, "D_bc", 32), (snw_bc, snw_d, "snw_bc", 2048),
                           (rb_bc, rb_d, "rb_bc", NE)):
        DMA("sp", (lambda dst, src, n: lambda e: e.dma_start(out=dst, in_=src.to_broadcast([128, n])))(dst, src, n), [], [k])
    V("act", lambda e: e.activation(out=A_bc, in_=A_bc, func=AF.Exp), ["A_bc"], ["A_bc"])
    V("dve", lambda e: e.tensor_scalar(out=A_bc, in0=A_bc, scalar1=-1.0, scalar2=None, op0=ALU.mult), ["A_bc"], ["A_bc"])

    for r0 in range(0, D, 128):
        DMA("pool", (lambda r0: lambda e: e.dma_start(out=winb_d[r0:r0 + 128, :], in_=win_d[r0:r0 + 128, :]))(r0), [], [("winb", r0)])
        DMA("pool", (lambda r0: lambda e: e.dma_start(out=wob_d[r0:r0 + 128, :], in_=wo_d[r0:r0 + 128, :]))(r0), [], [("wob", r0)])
    for r0 in range(0, 2048, 128):
        DMA("pool", (lambda r0: lambda e: e.dma_start(out=wsob_d[r0:r0 + 128, :], in_=wso_d[r0:r0 + 128, :]))(r0), [], [("wsob", r0)])
        DMA("pool", (lambda r0: lambda e: e.dma_start(out=wrob_d[r0:r0 + 128, :], in_=wro_d[r0:r0 + 128, :]))(r0), [], [("wrob", r0)])
    WINB_KEYS = [("winb", r0) for r0 in range(0, D, 128)]
    WOB_KEYS = [("wob", r0) for r0 in range(0, D, 128)]
    WSOB_KEYS = [("wsob", r0) for r0 in range(0, 2048, 128)]
    WROB_KEYS = [("wrob", r0) for r0 in range(0, 2048, 128)]

    if stop == 0:
        return locals()
    sc_col = A.alloc([8], F32)
    cols = A.alloc([32], F32)
    s1col = A.alloc([8], F32)
    sh1col = A.alloc([8], F32)
    s2col = A.alloc([8], F32)
    sh2col = A.alloc([8], F32)
    g1_bc = A.alloc([D], F32)
    g2_bc = A.alloc([D], F32)
    S_f = A.alloc([2048], F32)
    S_b = A.alloc([2048], BF16)
    R_f = A.alloc([8, 512], F32)
    R_b = A.alloc([8, 512], BF16)
    u = A.alloc([24, 131], F32)
    hT = A.alloc([8, 128], BF16)
    ynT = A.alloc([16, 128], BF16)
    yrT = A.alloc([16, 128], BF16)
    V("dve", lambda e: e.memset(S_f, 0.0), [], ["S_f"])
    V("dve", lambda e: e.memset(S_b, 0.0), [], ["S_b"])
    V("pool", lambda e: e.memset(R_f, 0.0), [], ["R_f"])
    V("pool", lambda e: e.memset(R_b, 0.0), [], ["R_b"])
    V("pool", lambda e: e.memset(u, 0.0), [], ["u"])
    striu_b = A.alloc([128], BF16)
    ones_b = A.alloc([128], BF16)
    iota_ei = A.alloc([NE], I32)
    iota_e = A.alloc([NE], F32)
    iota8k = A.alloc([NE], F32)
    run_bc = A.alloc([NE], F32)
    e8s = A.alloc([NCH, 8], F32)
    pos8s = A.alloc([NCH, 8], F32)
    w8s = A.alloc([NCH, 8], F32)
    V("dve", lambda e: e.tensor_single_scalar(striu_b, idf, 0.0, op=ALU.is_gt), ["idf"], ["striu_b"])
    V("dve", lambda e: e.memset(ones_b, 1.0), [], ["ones_b"])
    V("pool", lambda e: e.iota(iota_ei, pattern=[[1, NE]], base=0, channel_multiplier=0), [], ["iota_ei"])
    V("dve", lambda e: e.tensor_copy(iota_e, iota_ei), ["iota_ei"], ["iota_e"])
    V("dve", lambda e: e.tensor_scalar(out=iota8k, in0=iota_e, scalar1=8192.0, scalar2=None, op0=ALU.mult), ["iota_e"], ["iota8k"])
    V("dve", lambda e: e.memset(run_bc, 0.0), [], ["run_bc"])
    V("dve", lambda e: e.memset(w8s, 0.0), [], ["w8s"])
    MARK = A.off
    mod_row = A.alloc([6 * D], F32)
    bada = A.alloc([6 * D], F32)
    wa_buf = [A.alloc([8, 512], F32) for _ in range(2)]
    V("act", lambda e: e.activation(out=sc_col, in_=ccol, func=AF.Silu), ["ccol"], ["sc_col"])
    DMA("sp", lambda e: e.dma_start(out=bada[0:1, :], in_=bada_d), [], ["bada"])
    for nt in range(12):
        wb = wa_buf[nt % 2]
        kb = "wa%d" % (nt % 2)
        DMA("sp", (lambda wb, nt: lambda e: e.dma_start(out=wb, in_=wada_d[:, nt * 512:(nt + 1) * 512].rearrange("(k p) n -> p k n", p=128)))(wb, nt), [], [kb])
        for k in range(8):
            mm(psf[0][0:1, :], sc_col[:, k:k + 1], wb[:, k, :], k == 0, k == 7, [kb, "sc_col"], ["psf0"])
        V("dve", (lambda nt: lambda e: e.tensor_tensor(out=mod_row[0:1, nt * 512:(nt + 1) * 512], in0=psf[0][0:1, :], in1=bada[0:1, nt * 512:(nt + 1) * 512], op=ALU.add))(nt), ["psf0", "bada"], ["mod_row"])
    for qi, q in enumerate((0, 1, 3, 4)):
        for k in range(8):
            mm(psf[1][:, qi * 8 + k:qi * 8 + k + 1], mod_row[0:1, q * D + k * 128:q * D + (k + 1) * 128], ones_f[0:1, 0:1], True, True, ["mod_row", "ones_f"], ["psf1"])
    V("dve", lambda e: e.tensor_copy(cols, psf[1][:, 0:32]), ["psf1"], ["cols"])
    V("dve", lambda e: e.tensor_copy(sh1col, cols[:, 0:8]), ["cols"], ["sh1col"])
    V("dve", lambda e: e.scalar_tensor_tensor(out=s1col, in0=cols[:, 8:16], scalar=1.0, in1=n1col, op0=ALU.add, op1=ALU.mult), ["cols", "n1col"], ["s1col"])
    V("dve", lambda e: e.tensor_copy(sh2col, cols[:, 16:24]), ["cols"], ["sh2col"])
    V("dve", lambda e: e.scalar_tensor_tensor(out=s2col, in0=cols[:, 24:32], scalar=1.0, in1=n2col, op0=ALU.add, op1=ALU.mult), ["cols", "n2col"], ["s2col"])
    for gi, (gb, q, kk) in enumerate(((g1_bc, 2, "g1_bc"), (g2_bc, 5, "g2_bc"))):
        for hh in range(2):
            pp = psf[2 + hh]
            mm(pp[:, :], ones_f[0:1, :], mod_row[0:1, q * D + hh * 512:q * D + (hh + 1) * 512], True, True, ["mod_row", "ones_f"], ["psf%d" % (2 + hh)])
            V("dve", (lambda gb, hh, pp: lambda e: e.tensor_copy(gb[:, hh * 512:(hh + 1) * 512], pp[:, :]))(gb, hh, pp), ["psf%d" % (2 + hh)], [kk])

    if stop == 1:
        return locals()
    bscr = A.alloc([4], F32)
    bdr = dram("bdr", [1, 8], F32, kind="Internal")

    def barrier():
        P.barrier({
            "dve": (False, lambda e: e.memset(bscr[0:1, 0:1], 0.0)),
            "pool": (False, lambda e: e.memset(bscr[0:1, 1:2], 0.0)),
            "act": (False, lambda e: e.activation(out=bscr[0:1, 2:3], in_=ones_f[0:1, 0:1], func=AF.Copy)),
            "sp": (True, lambda e: e.dma_start(out=bdr[0:1, 0:4], in_=bdr[0:1, 4:8])),
        })

    barrier()
    A.off = MARK
    MARK2 = MARK

    NBW, LOOK = 4, 2
    wt = [A.alloc([8, 512], BF16) for _ in range(NBW)]
    SEQ = []
    for t0 in range(0, 2048, 512):
        SEQ.append(("winb", 0, OFF_Z + t0, 512))
    for t0 in range(0, 3072, 512):
        SEQ.append(("winb", 0, OFF_XBC + t0, 512))
    SEQ.append(("winb", 0, OFF_DT, 32))
    for off, n in ((OFF_Q, 1024), (OFF_K, 1024), (OFF_V, 2048), (OFF_G, 2048), (OFF_GS, 1024), (OFF_GR, 1024)):
        for t0 in range(0, n, 512):
            SEQ.append(("winb", 0, off + t0, 512))
    for nm in ("wsob", "wrob"):
        for nh in range(2):
            for kg in range(2):
                SEQ.append((nm, kg * 1024, nh * 512, 512))
    for nh in range(2):
        SEQ.append(("wob", 0, nh * 512, 512))
    WSRC = {"winb": (winb_d, WINB_KEYS), "wsob": (wsob_d, WSOB_KEYS), "wrob": (wrob_d, WROB_KEYS), "wob": (wob_d, WOB_KEYS)}
    ws_pos = [0]
    ws_issued = [0]
    WS_TOTAL = NCH * len(SEQ)

    def _issue_w(idx):
        nm, r0, c0, ncols = SEQ[idx % len(SEQ)]
        src_d, srckeys = WSRC[nm]
        i = idx % NBW
        b = wt[i]
        DMA("sp", lambda e: e.dma_start(out=b[:, :, 0:ncols], in_=src_d[r0:r0 + 1024, c0:c0 + ncols].rearrange("(k p) n -> p k n", p=128)), srckeys, ["wt%d" % i])

    def load_w(src_d, srckeys, r0, c0, ncols):
        j = ws_pos[0]
        nm, r0_, c0_, n_ = SEQ[j % len(SEQ)]
        assert WSRC[nm][0] is src_d and (r0_, c0_, n_) == (r0, c0, ncols), ("weight stream order mismatch", j, SEQ[j % len(SEQ)], r0, c0, ncols)
        while ws_issued[0] <= min(j + LOOK, WS_TOTAL - 1):
            _issue_w(ws_issued[0])
            ws_issued[0] += 1
        ws_pos[0] += 1
        return wt[j % NBW], "wt%d" % (j % NBW)

    pj_i = [0]

    def next_pj():
        i = pj_i[0] % 3
        pj_i[0] += 1
        return psf[i], "psf%d" % i

    def proj_tok(c0, ncols, evac):
        for t0 in range(0, ncols, 512):
            n = min(512, ncols - t0)
            b, bk = load_w(winb_d, WINB_KEYS, 0, c0 + t0, n)
            pp, pk = next_pj()
            for k in range(8):
                mm(pp[:, 0:n], hT[:, k, :], b[:, k, 0:n], k == 0, k == 7, ["hT", bk], [pk])
            evac(pp, pk, t0, n)

    def proj_feat(c0, ncols, evac):
        for t0 in range(0, ncols, 512):
            b, bk = load_w(winb_d, WINB_KEYS, 0, c0 + t0, 512)
            pp, pk = next_pj()
            for j in range(4):
                for k in range(8):
                    mm(pp[:, j * 128:(j + 1) * 128], b[:, k, j * 128:(j + 1) * 128], hT[:, k, :], k == 0, k == 7, ["hT", bk], [pk])
            evac(pp, pk, t0 // 128)

    qraw = A.alloc([8, 128], F32)
    kraw = A.alloc([8, 128], F32)
    vtok = A.alloc([2048], BF16)
    gsil = A.alloc([2048], BF16)
    sgT = A.alloc([8, 128], BF16)
    srT = A.alloc([8, 128], BF16)
    SCR0 = A.off
    for ch in range(NCH):
        A.off = SCR0
        tok0 = ch * 128
        xt = A.alloc([D], F32)
        xn = A.alloc([D], BF16)
        junk = A.alloc([D], BF16)
        ss = A.alloc([4], F32)
        DMA("sp", lambda e, tok0=tok0, xt=xt: e.dma_start(out=xt, in_=x_d[tok0:tok0 + 128, :]), [], ["xt"])
        V("dve", lambda e, ss=ss: e.memset(ss, 0.0), [], ["ss"])
        V("act", lambda e, xt=xt, junk=junk, ss=ss: e.activation(out=junk, in_=xt, func=AF.Square, accum_out=ss[:, 0:1]), ["xt", "ss"], ["junk", "ss"])
        V("dve", lambda e, ss=ss: e.tensor_scalar(out=ss[:, 1:2], in0=ss[:, 0:1], scalar1=1.0 / D, scalar2=EPS, op0=ALU.mult, op1=ALU.add), ["ss"], ["ss"])
        V("act", lambda e, ss=ss: e.activation(out=ss[:, 3:4], in_=ss[:, 1:2], func=AF.Ln), ["ss"], ["ss"])
        V("act", lambda e, ss=ss: e.activation(out=ss[:, 2:3], in_=ss[:, 3:4], func=AF.Exp, scale=-0.5), ["ss"], ["ss"])
        V("dve", lambda e, xt=xt, xn=xn, ss=ss: e.tensor_scalar(out=xn, in0=xt, scalar1=ss[:, 2:3], scalar2=None, op0=ALU.mult), ["xt", "ss"], ["xn"])
        pT = psb[0].rearrange("p (a b) -> p a b", a=8)
        for k in range(8):
            tr(pT[:, k, :], xn[:, k * 128:(k + 1) * 128], ident_b, ["xn", "ident_b"], ["psb0"])
        for k in range(8):
            V("act", lambda e, k=k, pT=pT: e.activation(out=hT[:, k, :], in_=pT[:, k, :], func=AF.Identity, bias=sh1col[:, k:k + 1], scale=s1col[:, k:k + 1]), ["psb0", "s1col", "sh1col"], ["hT"])

        if stop == 2:
            return locals()
        zs = A.alloc([2048], BF16)
        xc = A.alloc([24, 128], BF16)
        cacc = A.alloc([4, 128], F32)
        sm = A.alloc([8, 32], F32)
        xtok = A.alloc([2048], BF16)
        xdt = A.alloc([2048], BF16)
        Btok = A.alloc([512], BF16)
        Atri = A.alloc([8, 128], F32)
        dec = A.alloc([8, 128], BF16)
        wTt = A.alloc([8, 128], BF16)
        cbs = A.alloc([128], F32)
        t1 = A.alloc([512], F32)
        t2 = A.alloc([512], F32)
        yg = A.alloc([512], F32)
        yn = A.alloc([2048], BF16)
        gsm = A.alloc([16], F32)
        eal = A.alloc([8], F32)

        def ev_z(pp, pk, t0, n):
            V("act", lambda e: e.activation(out=zs[:, t0:t0 + n], in_=pp[:, 0:n], func=AF.Silu), [pk], ["zs"])
        proj_tok(OFF_Z, 2048, ev_z)

        def ev_xbc(pp, pk, j0):
            V("act", lambda e: e.copy(u[:, j0:j0 + 4, 3:131], pp[:, :].rearrange("p (a b) -> p a b", a=4)), [pk], ["u"])
        proj_feat(OFF_XBC, 3072, ev_xbc)

        def ev_dt(pp, pk, t0, n):
            V("dve", lambda e: e.tensor_tensor(out=sm[:, 0, :], in0=pp[:, 0:32], in1=dtb_bc, op=ALU.add), [pk, "dtb_bc"], ["sm"])
        proj_tok(OFF_DT, 32, ev_dt)
        V("act", lambda e: e.activation(out=sm[:, 6, :], in_=sm[:, 0, :], func=AF.Exp), ["sm"], ["sm"])
        V("act", lambda e: e.activation(out=sm[:, 1, :], in_=sm[:, 6, :], func=AF.Ln, bias=1.0), ["sm"], ["sm"])
        V("dve", lambda e: e.tensor_tensor(out=sm[:, 2, :], in0=sm[:, 1, :], in1=A_bc, op=ALU.mult), ["sm", "A_bc"], ["sm"])
        for j in range(24):
            cj = cacc[:, j % 4, :]
            ck = ("cacc", j % 4)
            V("dve", lambda e, j=j, cj=cj: e.tensor_scalar(out=cj, in0=u[:, j, 0:128], scalar1=convw[:, j, 0:1], scalar2=convb[:, j:j + 1], op0=ALU.mult, op1=ALU.add), ["u", "convw", "convb"], [ck])
            for t in range(1, 4):
                V("dve", lambda e, j=j, t=t, cj=cj: e.scalar_tensor_tensor(out=cj, in0=u[:, j, t:t + 128], scalar=convw[:, j, t:t + 1], in1=cj, op0=ALU.mult, op1=ALU.add), ["u", "convw", ck], [ck])
            V("act", lambda e, j=j, cj=cj: e.activation(out=xc[:, j, :], in_=cj, func=AF.Silu), [ck], ["xc"])
        V("pool", lambda e: e.tensor_copy(u[:, :, 0:3], u[:, :, 128:131]), ["u"], ["u"])
        if stop == 3:
            return locals()
        pX0 = psb[0].rearrange("p (a b) -> p a b", a=8)
        pX1 = psb[1].rearrange("p (a b) -> p a b", a=8)
        for j in range(8):
            tr(pX0[:, j, :], xc[:, j, :], ident_b, ["xc", "ident_b"], ["psb0"])
        for j in range(8):
            tr(pX1[:, j, :], xc[:, 8 + j, :], ident_b, ["xc", "ident_b"], ["psb1"])
        V("act", lambda e: e.copy(xtok[:, 0:1024], psb[0][:, :]), ["psb0"], ["xtok"])
        V("act", lambda e: e.copy(xtok[:, 1024:2048], psb[1][:, :]), ["psb1"], ["xtok"])
        if stop == 33:
            return locals()
        dtv_b = lambda h0, nh: sm[:, 1, h0:h0 + nh].unsqueeze(2).to_broadcast([128, nh, 64])
        V("dve", lambda e: e.tensor_tensor(out=xdt[:, 0:1024].rearrange("p (a b) -> p a b", a=16), in0=xtok[:, 0:1024].rearrange("p (a b) -> p a b", a=16), in1=dtv_b(0, 16), op=ALU.mult), ["xtok", "sm"], ["xdt"])
        V("dve", lambda e: e.tensor_tensor(out=xdt[:, 1024:2048].rearrange("p (a b) -> p a b", a=16), in0=xtok[:, 1024:2048].rearrange("p (a b) -> p a b", a=16), in1=dtv_b(16, 16), op=ALU.mult), ["xtok", "sm"], ["xdt"])
        for j in range(4):
            tr(pX0[:, j, :], xc[:, 16 + j, :], ident_b, ["xc", "ident_b"], ["psb0"])
        V("act", lambda e: e.copy(Btok, psb[0][:, 0:512]), ["psb0"], ["Btok"])
        if stop == 35:
            return locals()
        mm(psf[3][:, 0:32], tri_f, sm[:, 2, :], True, True, ["tri_f", "sm"], ["psf3"])
        V("dve", lambda e: e.tensor_copy(sm[:, 3, :], psf[3][:, 0:32]), ["psf3"], ["sm"])
        V("dve", lambda e: e.tensor_scalar(out=sm[:, 4, :], in0=sm[:, 3, :], scalar1=-1.0, scalar2=None, op0=ALU.mult), ["sm"], ["sm"])
        V("act", lambda e: e.activation(out=sm[:, 5, :], in_=sm[:, 3, :], func=AF.Exp), ["sm"], ["sm"])
        if stop == 4:
            return locals()
        for g in range(4):
            h0 = g * 8
            V("pool", lambda e, h0=h0: e.tensor_tensor(out=Atri, in0=tri_f.unsqueeze(1).to_broadcast([128, 8, 128]), in1=sm[:, 2, h0:h0 + 8].unsqueeze(2).to_broadcast([128, 8, 128]), op=ALU.mult), ["tri_f", "sm"], ["Atri"])
            segp = (psf[3], psf[4])
            for hh in range(2):
                mm(segp[hh][:, :], ones_f, Atri[:, hh * 4:(hh + 1) * 4, :].rearrange("p a b -> p (a b)"), True, False, ["ones_f", "Atri"], ["psf%d" % (3 + hh)])
                mm(segp[hh][:, :], ident_b, negm8[:, 0:4, :].rearrange("p a b -> p (a b)"), False, True, ["ident_b", "negm8"], ["psf%d" % (3 + hh)])
            if stop == 5:
                return locals()
            mm(psf[5][:, 0:128], xc[:, 16 + g, :], xc[:, 20 + g, :], True, True, ["xc"], ["psf5"])
            V("dve", lambda e: e.tensor_copy(cbs, psf[5][:, 0:128]), ["psf5"], ["cbs"])
            for hl in range(8):
                sp_ = segp[hl // 4]
                V("act", lambda e, hl=hl, sp_=sp_, h0=h0: e.activation(out=dec[:, hl, :], in_=sp_[:, (hl % 4) * 128:(hl % 4 + 1) * 128], func=AF.Exp, bias=sm[:, 4, h0 + hl:h0 + hl + 1], scale=1.0), ["psf%d" % (3 + hl // 4), "sm"], ["dec"])
            for hh in range(2):
                V("act", lambda e, hh=hh: e.activation(out=eal[:, hh * 4:(hh + 1) * 4], in_=segp[hh][:, :].rearrange("p (a b) -> p a b", a=4)[:, :, 127], func=AF.Exp), ["psf%d" % (3 + hh)], ["eal"])
                V("dve", lambda e, hh=hh, h0=h0: e.tensor_tensor(out=sm[:, 6, h0 + hh * 4:h0 + hh * 4 + 4], in0=segp[hh][:, :].rearrange("p (a b) -> p a b", a=4)[:, :, 127], in1=sm[:, 4, h0 + hh * 4:h0 + hh * 4 + 4], op=ALU.add), ["psf%d" % (3 + hh), "sm"], ["sm"])
            V("act", lambda e, h0=h0: e.activation(out=sm[:, 7, h0:h0 + 8], in_=sm[:, 6, h0:h0 + 8], func=AF.Exp), ["sm"], ["sm"])
            V("dve", lambda e: e.tensor_tensor(out=wTt, in0=dec, in1=cbs.unsqueeze(1).to_broadcast([128, 8, 128]), op=ALU.mult), ["dec", "cbs"], ["wTt"])
            for hl in range(8):
                mm(psf[5][:, hl * 64:(hl + 1) * 64], wTt[:, hl, :], xdt[:, (h0 + hl) * 64:(h0 + hl + 1) * 64], True, True, ["wTt", "xdt", "cbs"], ["psf5"])
            mm(psf[3][:, :], xc[:, 20 + g, :], S_b[:, g * 512:(g + 1) * 512], True, True, ["xc", "S_b", "dec", "eal", "sm"], ["psf3"])
            V("act", lambda e: e.copy(t1, psf[3][:, :]), ["psf3"], ["t1"])
            V("dve", lambda e, h0=h0: e.tensor_tensor(out=t1.rearrange("p (a b) -> p a b", a=8), in0=t1.rearrange("p (a b) -> p a b", a=8), in1=sm[:, 5, h0:h0 + 8].unsqueeze(2).to_broadcast([128, 8, 64]), op=ALU.mult), ["t1", "sm"], ["t1"])
            V("pool", lambda e, h0=h0, g=g: e.tensor_tensor(out=t2.rearrange("p (a b) -> p a b", a=8), in0=xtok[:, g * 512:(g + 1) * 512].rearrange("p (a b) -> p a b", a=8), in1=D_bc[:, h0:h0 + 8].unsqueeze(2).to_broadcast([128, 8, 64]), op=ALU.mult), ["xtok", "D_bc"], ["t2"])
            V("pool", lambda e: e.tensor_tensor(out=t2, in0=t2, in1=t1, op=ALU.add), ["t1", "t2"], ["t2"])
            V("dve", lambda e: e.tensor_tensor(out=yg, in0=psf[5][:, :], in1=t2, op=ALU.add), ["psf5", "t2"], ["yg"])
            V("dve", lambda e, g=g: e.tensor_tensor(out=yg, in0=yg, in1=zs[:, g * 512:(g + 1) * 512], op=ALU.mult), ["yg", "zs"], ["yg"])
            V("dve", lambda e: e.memset(gsm, 0.0), [], ["gsm"])
            V("act", lambda e: e.activation(out=t1, in_=yg, func=AF.Square, accum_out=gsm[:, 0:1]), ["yg", "gsm", "t1"], ["t1", "gsm"])
            V("dve", lambda e: e.tensor_scalar(out=gsm[:, 1:2], in0=gsm[:, 0:1], scalar1=1.0 / 512, scalar2=EPS, op0=ALU.mult, op1=ALU.add), ["gsm"], ["gsm"])
            V("act", lambda e: e.activation(out=gsm[:, 3:4], in_=gsm[:, 1:2], func=AF.Ln), ["gsm"], ["gsm"])
            V("act", lambda e: e.activation(out=gsm[:, 2:3], in_=gsm[:, 3:4], func=AF.Exp, scale=-0.5), ["gsm"], ["gsm"])
            V("dve", lambda e, g=g: e.scalar_tensor_tensor(out=yn[:, g * 512:(g + 1) * 512], in0=yg, scalar=gsm[:, 2:3], in1=snw_bc[:, g * 512:(g + 1) * 512], op0=ALU.mult, op1=ALU.mult), ["yg", "gsm", "snw_bc"], ["yn"])
            V("pool", lambda e, g=g, h0=h0: e.tensor_tensor(out=xdt[:, g * 512:(g + 1) * 512].rearrange("p (a b) -> p a b", a=8), in0=xdt[:, g * 512:(g + 1) * 512].rearrange("p (a b) -> p a b", a=8), in1=sm[:, 7, h0:h0 + 8].unsqueeze(2).to_broadcast([128, 8, 64]), op=ALU.mult), ["xdt", "sm", "psf5"], ["xdt"])
            mm(psf[4][:, :], Btok[:, g * 128:(g + 1) * 128], xdt[:, g * 512:(g + 1) * 512], True, True, ["Btok", "xdt", "eal", "dec"], ["psf4"])
            V("dve", lambda e, g=g: e.tensor_tensor(out=S_f[:, g * 512:(g + 1) * 512].rearrange("p (a b) -> p a b", a=8), in0=S_f[:, g * 512:(g + 1) * 512].rearrange("p (a b) -> p a b", a=8), in1=eal.unsqueeze(2).to_broadcast([128, 8, 64]), op=ALU.mult), ["S_f", "eal"], ["S_f"])
            V("dve", lambda e, g=g: e.tensor_tensor(out=S_f[:, g * 512:(g + 1) * 512], in0=S_f[:, g * 512:(g + 1) * 512], in1=psf[4][:, :], op=ALU.add), ["S_f", "psf4", "psf3"], ["S_f"])
            V("act", lambda e, g=g: e.copy(S_b[:, g * 512:(g + 1) * 512], S_f[:, g * 512:(g + 1) * 512]), ["S_f", "psf3"], ["S_b"])
            if g == 0:
                def ev_q(pp, pk, j0):
                    V("act", lambda e: e.copy(qraw[:, j0:j0 + 4, :], pp[:, :].rearrange("p (a b) -> p a b", a=4)), [pk], ["qraw"])
                proj_feat(OFF_Q, 1024, ev_q)
                def ev_k(pp, pk, j0):
                    V("act", lambda e: e.mul(kraw[:, j0:j0 + 4, :], pp[:, :].rearrange("p (a b) -> p a b", a=4), 1.0 / 16.0), [pk], ["kraw"])
                proj_feat(OFF_K, 1024, ev_k)
            if g == 1:
                def ev_v(pp, pk, t0, n):
                    V("act", lambda e: e.copy(vtok[:, t0:t0 + n], pp[:, 0:n]), [pk], ["vtok"])
                proj_tok(OFF_V, 2048, ev_v)
            if g == 2:
                def ev_g(pp, pk, t0, n):
                    V("act", lambda e: e.activation(out=gsil[:, t0:t0 + n], in_=pp[:, 0:n], func=AF.Silu), [pk], ["gsil"])
                proj_tok(OFF_G, 2048, ev_g)
            if g == 3:
                def ev_gs(pp, pk, j0):
                    V("act", lambda e: e.activation(out=sgT[:, j0:j0 + 4, :], in_=pp[:, :].rearrange("p (a b) -> p a b", a=4), func=AF.Sigmoid), [pk], ["sgT"])
                proj_feat(OFF_GS, 1024, ev_gs)
                def ev_gr(pp, pk, j0):
                    V("act", lambda e: e.activation(out=srT[:, j0:j0 + 4, :], in_=pp[:, :].rearrange("p (a b) -> p a b", a=4), func=AF.Sigmoid), [pk], ["srT"])
                proj_feat(OFF_GR, 1024, ev_gr)
        for hh in range(2):
            pX = psb[hh].rearrange("p (a b) -> p a b", a=8)
            for j in range(8):
                tr(pX[:, j, :], yn[:, (hh * 8 + j) * 128:(hh * 8 + j + 1) * 128], ident_b, ["yn", "ident_b", "xtok", "xdt", "Btok"], ["psb%d" % hh])
            V("act", lambda e, hh=hh: e.copy(ynT[:, hh * 8:(hh + 1) * 8, :].rearrange("p a b -> p (a b)"), psb[hh][:, :]), ["psb%d" % hh], ["ynT"])
        if dbg and ch == NCH - 1:
            dbg_d["yn"] = dram("dbg_yn", [128, 2048], BF16, kind="ExternalOutput")
            DMA("sp", lambda e: e.dma_start(out=dbg_d["yn"], in_=yn), ["yn"], [])
        barrier()
        A.off = SCR0
        qT = A.alloc([8, 128], BF16)
        kT = A.alloc([8, 128], BF16)
        rt = [A.alloc([4, 128], F32) for _ in range(4)]
        posi = A.alloc([128], I32)
        ang = A.alloc([128], F32)
        rr = A.alloc([128], F32)
        cosT = A.alloc([128], F32)
        sinT = A.alloc([128], F32)
        ktd = A.alloc([1024], BF16)
        sTd = A.alloc([4, 128], BF16)
        qdT = A.alloc([8, 128], BF16)
        yr = A.alloc([2048], BF16)
        ybuf = A.alloc([512], F32)
        ysq = A.alloc([512], F32)
        rs = A.alloc([8], F32)


        DMA("sp", lambda e, tok0=tok0: e.dma_start(out=posi, in_=pos_d[0:1, tok0:tok0 + 128].to_broadcast([128, 128])), [], ["posi"])
        V("dve", lambda e: e.tensor_copy(ang, posi), ["posi"], ["ang"])
        V("dve", lambda e: e.tensor_scalar(out=ang, in0=ang, scalar1=invf[:, 0:1], scalar2=None, op0=ALU.mult), ["ang", "invf"], ["ang"])
        V("dve", lambda e: e.tensor_scalar(out=rr, in0=ang, scalar1=1.0 / (2 * math.pi), scalar2=None, op0=ALU.mult), ["ang"], ["rr"])
        V("dve", lambda e: e.tensor_copy(posi, rr), ["rr", "ang"], ["posi"])
        V("dve", lambda e: e.tensor_copy(rr, posi), ["posi"], ["rr"])
        V("dve", lambda e: e.scalar_tensor_tensor(out=rr, in0=rr, scalar=-2 * math.pi, in1=ang, op0=ALU.mult, op1=ALU.add), ["rr", "ang"], ["rr"])
        V("dve", lambda e: e.tensor_scalar(out=rr, in0=rr, scalar1=3.1415925, scalar2=-3.1415925, op0=ALU.min, op1=ALU.max), ["rr"], ["rr"])
        V("act", lambda e: e.activation(out=sinT, in_=rr, func=AF.Sin), ["rr"], ["sinT"])
        V("dve", lambda e: e.tensor_scalar(out=ang, in0=ang, scalar1=0.5 * math.pi, scalar2=None, op0=ALU.add), ["ang", "sinT", "rr"], ["ang"])
        V("dve", lambda e: e.tensor_scalar(out=rr, in0=ang, scalar1=1.0 / (2 * math.pi), scalar2=None, op0=ALU.mult), ["ang", "sinT"], ["rr"])
        V("dve", lambda e: e.tensor_copy(posi, rr), ["rr"], ["posi"])
        V("dve", lambda e: e.tensor_copy(rr, posi), ["posi"], ["rr"])
        V("dve", lambda e: e.scalar_tensor_tensor(out=rr, in0=rr, scalar=-2 * math.pi, in1=ang, op0=ALU.mult, op1=ALU.add), ["rr", "ang"], ["rr"])
        V("dve", lambda e: e.tensor_scalar(out=rr, in0=rr, scalar1=3.1415925, scalar2=-3.1415925, op0=ALU.min, op1=ALU.max), ["rr"], ["rr"])
        V("act", lambda e: e.activation(out=cosT, in_=rr, func=AF.Sin), ["rr"], ["cosT"])
        cos_b = cosT.unsqueeze(1).to_broadcast([128, 4, 128])
        sin_b = sinT.unsqueeze(1).to_broadcast([128, 4, 128])
        for raw, outT, rk, ok in ((qraw, qT, "qraw", "qT"), (kraw, kT, "kraw", "kT")):
            rv = raw.rearrange("p (h t) n -> p h t n", t=2)
            ov = outT.rearrange("p (h t) n -> p h t n", t=2)
            V("dve", lambda e, rv=rv: e.tensor_tensor(out=rt[0], in0=rv[:, :, 0, :], in1=cos_b, op=ALU.mult), [rk, "cosT", "rt0"], ["rt0"])
            V("pool", lambda e, rv=rv: e.tensor_tensor(out=rt[1], in0=rv[:, :, 1, :], in1=sin_b, op=ALU.mult), [rk, "sinT", "rt1"], ["rt1"])
            V("dve", lambda e, ov=ov: e.tensor_tensor(out=ov[:, :, 0, :], in0=rt[0], in1=rt[1], op=ALU.subtract), ["rt0", "rt1"], [ok])
            V("pool", lambda e, rv=rv: e.tensor_tensor(out=rt[2], in0=rv[:, :, 0, :], in1=sin_b, op=ALU.mult), [rk, "sinT", "rt2"], ["rt2"])
            V("dve", lambda e, rv=rv: e.tensor_tensor(out=rt[3], in0=rv[:, :, 1, :], in1=cos_b, op=ALU.mult), [rk, "cosT", "rt3"], ["rt3"])
            V("dve", lambda e, ov=ov: e.tensor_tensor(out=ov[:, :, 1, :], in0=rt[2], in1=rt[3], op=ALU.add), ["rt2", "rt3"], [ok])
        pX0 = psb[0].rearrange("p (a b) -> p a b", a=8)
        for j in range(8):
            tr(pX0[:, j, :], kT[:, j, :], ident_b, ["kT", "ident_b"], ["psb0"])
        V("act", lambda e: e.copy(ktd, psb[0][:, :]), ["psb0"], ["ktd"])
        V("dve", lambda e: e.tensor_tensor(out=ktd.rearrange("p (a b) -> p a b", a=4), in0=ktd.rearrange("p (a b) -> p a b", a=4), in1=kdec.unsqueeze(2).to_broadcast([128, 4, 256]), op=ALU.mult), ["ktd", "kdec"], ["ktd"])
        qv = qT.rearrange("p (h t) n -> p h t n", t=2)
        qdv = qdT.rearrange("p (h t) n -> p h t n", t=2)
        for t in range(2):
            V("pool", lambda e, t=t: e.tensor_tensor(out=qdv[:, :, t, :], in0=qv[:, :, t, :], in1=qdec_bc, op=ALU.mult), ["qT", "qdec_bc"], ["qdT"])
        for h in range(4):
            for j in range(2):
                mm(psf[3][:, h * 128:(h + 1) * 128], kT[:, 2 * h + j, :], qT[:, 2 * h + j, :], j == 0, j == 1, ["kT", "qT"], ["psf3"])
        V("dve", lambda e: e.tensor_tensor(out=sTd, in0=psf[3][:, :].rearrange("p (a b) -> p a b", a=4), in1=idecT, op=ALU.mult), ["psf3", "idecT"], ["sTd"])
        for h in range(4):
            mm(psf[4][:, :], sTd[:, h, :], vtok[:, h * 512:(h + 1) * 512], True, False, ["sTd", "vtok"], ["psf4"])
            for j in range(2):
                mm(psf[4][:, :], qdT[:, 2 * h + j, :], R_b[:, 2 * h + j, :], False, j == 1, ["qdT", "R_b"], ["psf4"])
            V("dve", lambda e: e.memset(rs, 0.0), [], ["rs"])
            V("act", lambda e: e.activation(out=ybuf, in_=psf[4][:, :], func=AF.Identity, accum_out=rs[:, 0:1]), ["psf4", "rs"], ["ybuf", "rs"])
            V("act", lambda e: e.activation(out=ysq, in_=ybuf, func=AF.Square, accum_out=rs[:, 1:2]), ["ybuf", "rs"], ["ysq", "rs"])
            V("dve", lambda e: e.tensor_scalar(out=rs[:, 2:3], in0=rs[:, 0:1], scalar1=1.0 / 512, scalar2=None, op0=ALU.mult), ["rs"], ["rs"])
            V("dve", lambda e: e.tensor_tensor(out=rs[:, 3:4], in0=rs[:, 2:3], in1=rs[:, 2:3], op=ALU.mult), ["rs"], ["rs"])
            V("dve", lambda e: e.scalar_tensor_tensor(out=rs[:, 4:5], in0=rs[:, 1:2], scalar=1.0 / 512, in1=rs[:, 3:4], op0=ALU.mult, op1=ALU.subtract), ["rs"], ["rs"])
            V("dve", lambda e: e.tensor_scalar(out=rs[:, 4:5], in0=rs[:, 4:5], scalar1=EPS, scalar2=None, op0=ALU.add), ["rs"], ["rs"])
            V("act", lambda e: e.activation(out=rs[:, 5:6], in_=rs[:, 4:5], func=AF.Ln), ["rs"], ["rs"])
            V("act", lambda e: e.activation(out=rs[:, 5:6], in_=rs[:, 5:6], func=AF.Exp, scale=-0.5), ["rs"], ["rs"])
            V("dve", lambda e: e.scalar_tensor_tensor(out=rs[:, 6:7], in0=rs[:, 2:3], scalar=-1.0, in1=rs[:, 5:6], op0=ALU.mult, op1=ALU.mult), ["rs"], ["rs"])
            V("act", lambda e: e.activation(out=ysq, in_=ybuf, func=AF.Identity, bias=rs[:, 6:7], scale=rs[:, 5:6]), ["ybuf", "rs", "ysq"], ["ysq"])
            V("dve", lambda e, h=h: e.tensor_tensor(out=yr[:, h * 512:(h + 1) * 512], in0=ysq, in1=gsil[:, h * 512:(h + 1) * 512], op=ALU.mult), ["ysq", "gsil"], ["yr"])
            for j in range(2):
                mm(psf[5][:, :], ktd[:, h * 256 + j * 128:h * 256 + (j + 1) * 128], vtok[:, h * 512:(h + 1) * 512], True, True, ["ktd", "vtok"], ["psf5"])
                V("dve", lambda e, h=h, j=j: e.scalar_tensor_tensor(out=R_f[:, 2 * h + j, :], in0=R_f[:, 2 * h + j, :], scalar=cdec[h], in1=psf[5][:, :], op0=ALU.mult, op1=ALU.add), ["R_f", "psf5"], ["R_f"])
                V("act", lambda e, h=h, j=j: e.copy(R_b[:, 2 * h + j, :], R_f[:, 2 * h + j, :]), ["R_f", "psf4"], ["R_b"])
        for hh in range(2):
            pX = psb[hh].rearrange("p (a b) -> p a b", a=8)
            for j in range(8):
                tr(pX[:, j, :], yr[:, (hh * 8 + j) * 128:(hh * 8 + j + 1) * 128], ident_b, ["yr", "ident_b", "ktd"], ["psb%d" % hh])
            V("act", lambda e, hh=hh: e.copy(yrT[:, hh * 8:(hh + 1) * 8, :].rearrange("p a b -> p (a b)"), psb[hh][:, :]), ["psb%d" % hh], ["yrT"])
        if dbg and ch == NCH - 1:
            dbg_d["yr"] = dram("dbg_yr", [128, 2048], BF16, kind="ExternalOutput")
            DMA("sp", lambda e: e.dma_start(out=dbg_d["yr"], in_=yr), ["yr"], [])
        barrier()
        A.off = SCR0
        mA = A.alloc([8, 128], F32)
        mB = A.alloc([8, 128], F32)
        mT = A.alloc([8, 128], BF16)
        xt2 = A.alloc([D], F32)
        x2 = A.alloc([D], F32)
        xn2 = A.alloc([D], F32)
        h2f = A.alloc([8, 128], F32)
        h2b = A.alloc([8, 128], BF16)
        ss2 = A.alloc([4], F32)
        scr_ = A.alloc([NE], F32)
        sel = A.alloc([NE], F32)
        selm = A.alloc([NE], F32)
        wsel = A.alloc([NE], F32)
        m8all = A.alloc([8, 8], F32)
        gsc = A.alloc([8], F32)
        m8 = A.alloc([8], F32)
        gmask = A.alloc([8], F32)
        gm10 = A.alloc([8], F32)
        den = A.alloc([2], F32)


        for which, (wsrc, wkeys, inT, ink, gT, gk) in enumerate(((wsob_d, WSOB_KEYS, ynT, "ynT", sgT, "sgT"), (wrob_d, WROB_KEYS, yrT, "yrT", srT, "srT"))):
            for nh in range(2):
                pp, pk = next_pj()
                bb = [load_w(wsrc, wkeys, kg * 1024, nh * 512, 512) for kg in range(2)]
                for j in range(4):
                    for kg in range(2):
                        b, bk = bb[kg]
                        for k in range(8):
                            mm(pp[:, j * 128:(j + 1) * 128], b[:, k, j * 128:(j + 1) * 128], inT[:, kg * 8 + k, :], kg == 0 and k == 0, kg == 1 and k == 7, [ink, bk], [pk])
                dst = mA if which == 0 else mB
                V("dve", lambda e, dst=dst, pp=pp, gT=gT, nh=nh: e.tensor_tensor(out=dst[:, nh * 4:(nh + 1) * 4, :], in0=pp[:, :].rearrange("p (a b) -> p a b", a=4), in1=gT[:, nh * 4:(nh + 1) * 4, :], op=ALU.mult), [pk, gk], ["mA" if which == 0 else "mB"])
        V("dve", lambda e: e.tensor_tensor(out=mT, in0=mA, in1=mB, op=ALU.add), ["mA", "mB"], ["mT"])
        DMA("sp", lambda e, tok0=tok0: e.dma_start(out=xt2, in_=x_d[tok0:tok0 + 128, :]), [], ["xt2"])
        for nh in range(2):
            b, bk = load_w(wob_d, WOB_KEYS, 0, nh * 512, 512)
            pp, pk = next_pj()
            for k in range(8):
                mm(pp[:, :], mT[:, k, :], b[:, k, :], k == 0, k == 7, ["mT", bk], [pk])
            V("dve", lambda e, pp=pp, nh=nh: e.tensor_tensor(out=x2[:, nh * 512:(nh + 1) * 512], in0=pp[:, :], in1=g1_bc[:, nh * 512:(nh + 1) * 512], op=ALU.mult), [pk, "g1_bc"], ["x2"])
        V("dve", lambda e: e.tensor_tensor(out=x2, in0=x2, in1=xt2, op=ALU.add), ["x2", "xt2"], ["x2"])
        DMA("sp", lambda e, tok0=tok0: e.dma_start(out=x2_d[tok0:tok0 + 128, :], in_=x2), ["x2"], [("x2d", ch)])
        if dbg and ch == NCH - 1:
            dbg_d["x2"] = dram("dbg_x2", [128, D], F32, kind="ExternalOutput")
            DMA("sp", lambda e: e.dma_start(out=dbg_d["x2"], in_=x2), ["x2"], [])
            dbg_d["mT"] = dram("dbg_mT", [128, 8, 128], BF16, kind="ExternalOutput")
            DMA("sp", lambda e: e.dma_start(out=dbg_d["mT"], in_=mT), ["mT"], [])
        V("dve", lambda e: e.memset(ss2, 0.0), [], ["ss2"])
        V("act", lambda e: e.activation(out=xn2, in_=x2, func=AF.Square, accum_out=ss2[:, 0:1]), ["x2", "ss2"], ["xn2", "ss2"])
        V("dve", lambda e: e.tensor_scalar(out=ss2[:, 1:2], in0=ss2[:, 0:1], scalar1=1.0 / D, scalar2=EPS, op0=ALU.mult, op1=ALU.add), ["ss2"], ["ss2"])
        V("act", lambda e: e.activation(out=ss2[:, 3:4], in_=ss2[:, 1:2], func=AF.Ln), ["ss2"], ["ss2"])
        V("act", lambda e: e.activation(out=ss2[:, 2:3], in_=ss2[:, 3:4], func=AF.Exp, scale=-0.5), ["ss2"], ["ss2"])
        V("dve", lambda e: e.tensor_scalar(out=xn2, in0=x2, scalar1=ss2[:, 2:3], scalar2=None, op0=ALU.mult), ["x2", "ss2", "xn2"], ["xn2"])
        for k in range(8):
            tr(psf[3 + k // 4][:, (k % 4) * 128:(k % 4 + 1) * 128], xn2[:, k * 128:(k + 1) * 128], ident_f, ["xn2", "ident_f"], ["psf%d" % (3 + k // 4)])
        for k in range(8):
            V("act", lambda e, k=k: e.activation(out=h2f[:, k, :], in_=psf[3 + k // 4][:, (k % 4) * 128:(k % 4 + 1) * 128], func=AF.Identity, bias=sh2col[:, k:k + 1], scale=s2col[:, k:k + 1]), ["psf%d" % (3 + k // 4), "s2col", "sh2col"], ["h2f"])
        V("dve", lambda e: e.tensor_copy(h2b, h2f), ["h2f"], ["h2b"])
        DMA("sp", lambda e, tok0=tok0: e.dma_start(out=h2T_d[:, :, tok0:tok0 + 128], in_=h2b), ["h2b"], [("h2Td", ch)])
        wr_sb = A.alloc([8, NE], F32)
        DMA("sp", lambda e, wr_sb=wr_sb: e.dma_start(out=wr_sb, in_=wr_d.rearrange("(k p) n -> p k n", p=128)), [], ["wr_sb"])
        for k in range(8):
            mm(psf[5][:, 0:NE], h2f[:, k, :], wr_sb[:, k, :], k == 0, k == 7, ["h2f", "wr_sb"], ["psf5"])
        V("act", lambda e: e.activation(out=scr_, in_=psf[5][:, 0:NE], func=AF.Sigmoid), ["psf5"], ["scr_"])
        V("dve", lambda e: e.tensor_tensor(out=sel, in0=scr_, in1=rb_bc, op=ALU.add), ["scr_", "rb_bc"], ["sel"])
        for g in range(8):
            V("dve", lambda e, g=g: e.max(out=m8all[:, g, :], in_=sel[:, g * 32:(g + 1) * 32]), ["sel"], ["m8all"])
        V("dve", lambda e: e.tensor_tensor(out=gsc, in0=m8all[:, :, 0], in1=m8all[:, :, 1], op=ALU.add), ["m8all"], ["gsc"])
        V("dve", lambda e: e.max(out=m8, in_=gsc), ["gsc"], ["m8"])
        V("dve", lambda e: e.tensor_scalar(out=gmask, in0=gsc, scalar1=m8[:, 3:4], scalar2=None, op0=ALU.is_ge), ["gsc", "m8"], ["gmask"])
        V("dve", lambda e: e.tensor_scalar(out=gm10, in0=gmask, scalar1=10.0, scalar2=-10.0, op0=ALU.mult, op1=ALU.add), ["gmask"], ["gm10"])
        V("dve", lambda e: e.tensor_tensor(out=selm.rearrange("p (a b) -> p a b", a=8), in0=sel.rearrange("p (a b) -> p a b", a=8), in1=gmask.unsqueeze(2).to_broadcast([128, 8, 32]), op=ALU.mult), ["sel", "gmask"], ["selm"])
        V("dve", lambda e: e.tensor_tensor(out=selm.rearrange("p (a b) -> p a b", a=8), in0=selm.rearrange("p (a b) -> p a b", a=8), in1=gm10.unsqueeze(2).to_broadcast([128, 8, 32]), op=ALU.add), ["selm", "gm10"], ["selm"])
        V("dve", lambda e: e.max(out=m8, in_=selm), ["selm", "gmask"], ["m8"])
        V("dve", lambda e: e.memset(den, 0.0), [], ["den"])
        V("dve", lambda e: e.scalar_tensor_tensor(out=wsel, in0=selm, scalar=m8[:, 7:8], in1=scr_, op0=ALU.is_ge, op1=ALU.mult, accum_out=den[:, 0:1]), ["selm", "m8", "scr_", "den"], ["wsel", "den"])
        V("dve", lambda e: e.tensor_scalar(out=den[:, 1:2], in0=den[:, 0:1], scalar1=1e-20, scalar2=None, op0=ALU.add), ["den"], ["den"])
        V("dve", lambda e: e.reciprocal(den[:, 1:2], den[:, 1:2]), ["den"], ["den"])
        V("dve", lambda e: e.tensor_scalar(out=wsel, in0=wsel, scalar1=den[:, 1:2], scalar2=2.5, op0=ALU.mult, op1=ALU.mult), ["wsel", "den"], ["wsel"])
        Mf = A.alloc([NE], F32)
        Mb = A.alloc([NE], BF16)
        posf = A.alloc([NE], F32)
        keyf = A.alloc([NE], F32)
        kjunk = A.alloc([NE], F32)
        k8 = A.alloc([8], F32)
        k8i = A.alloc([8], I32)
        t8i = A.alloc([8], I32)
        t8j = A.alloc([8], I32)
        h2tok = A.alloc([D], BF16)
        V("dve", lambda e: e.tensor_single_scalar(Mf, wsel, 0.0, op=ALU.is_gt), ["wsel"], ["Mf"])
        V("dve", lambda e: e.tensor_copy(Mb, Mf), ["Mf"], ["Mb"])
        mm(psf[3][:, 0:NE], striu_b, Mb, True, True, ["striu_b", "Mb"], ["psf3"])
        mm(psf[4][:, 0:NE], ones_b, Mb, True, True, ["ones_b", "Mb"], ["psf4"])
        V("dve", lambda e: e.tensor_tensor(out=posf, in0=psf[3][:, 0:NE], in1=run_bc, op=ALU.add), ["psf3", "run_bc"], ["posf"])
        V("dve", lambda e: e.tensor_tensor(out=run_bc, in0=psf[4][:, 0:NE], in1=run_bc, op=ALU.add), ["psf4", "run_bc"], ["run_bc"])
        V("dve", lambda e: e.tensor_tensor(out=keyf, in0=posf, in1=iota8k, op=ALU.add), ["posf", "iota8k"], ["keyf"])
        V("dve", lambda e: e.scalar_tensor_tensor(out=keyf, in0=keyf, scalar=1.0, in1=Mf, op0=ALU.add, op1=ALU.mult), ["keyf", "Mf"], ["keyf"])
        V("dve", lambda e: e.tensor_scalar(out=keyf, in0=keyf, scalar1=-1.0, scalar2=None, op0=ALU.add), ["keyf"], ["keyf"])
        V("dve", lambda e: e.max(out=k8, in_=keyf), ["keyf"], ["k8"])
        V("dve", lambda e: e.tensor_copy(k8i, k8), ["k8"], ["k8i"])
        V("dve", lambda e: e.tensor_scalar(out=t8i, in0=k8i, scalar1=13, scalar2=None, op0=ALU.arith_shift_right), ["k8i"], ["t8i"])
        V("dve", lambda e, ch=ch: e.tensor_copy(e8s[:, ch, :], t8i), ["t8i"], ["e8s"])
        V("dve", lambda e: e.tensor_scalar(out=t8j, in0=k8i, scalar1=8191, scalar2=None, op0=ALU.bitwise_and), ["k8i"], ["t8j"])
        V("dve", lambda e, ch=ch: e.tensor_copy(pos8s[:, ch, :], t8j), ["t8j"], ["pos8s"])
        for k in range(8):
            V("dve", lambda e, k=k, ch=ch: e.scalar_tensor_tensor(out=kjunk, in0=keyf, scalar=k8[:, k:k + 1], in1=wsel, op0=ALU.is_equal, op1=ALU.mult, accum_out=w8s[:, ch, k:k + 1]), ["keyf", "k8", "wsel", "w8s", "kjunk"], ["kjunk", "w8s"])
        pXh = psb[0].rearrange("p (a b) -> p a b", a=8)
        for k in range(8):
            tr(pXh[:, k, :], h2b[:, k, :], ident_b, ["h2b", "ident_b"], ["psb0"])
        V("act", lambda e: e.copy(h2tok, psb[0][:, :]), ["psb0"], ["h2tok"])
        DMA("sp", lambda e, tok0=tok0: e.dma_start(out=h2tok_d[tok0:tok0 + 128, :], in_=h2tok), ["h2tok"], [("h2tokd", ch)])
        barrier()
        A.off = SCR0

    A.off = MARK2
    IO = lambda ap: bass.IndirectOffsetOnAxis(ap=ap, axis=0)
    widx_i = A.alloc([NB], I32)
    gidx = A.alloc([NB], I32)
    ridx = A.alloc([NB], I32)
    wcol_raw = A.alloc([NB], I32)
    wcol = wcol_raw.bitcast(F32)
    MARK3 = A.off
    padf = A.alloc([NE], F32)
    padi = A.alloc([NE], I32)
    pend = A.alloc([NE], F32)
    pstart = A.alloc([NE], F32)
    onesrow = A.alloc([NE], F32)
    Dk = [A.alloc([128], F32) for _ in range(2)]
    pcol2 = A.alloc([2], F32)
    bvals_i = A.alloc([NB], I32)
    bvals = A.alloc([NB], F32)
    ind = [A.alloc([NB], BF16) for _ in range(2)]
    widx_f = A.alloc([NB], F32)
    tabtmp = A.alloc([NB], I32)
    tab0 = A.alloc([NB, 2], I32)
    zrow = A.alloc([D], BF16)
    pst8s = A.alloc([NCH, 8], F32)
    slot_f = A.alloc([NCH, 8], F32)
    slot_i = A.alloc([NCH, 8], I32)
    sp_i = A.alloc([NCH, 8], I32)
    sb_i = A.alloc([NCH, 8], I32)
    sp_f = A.alloc([NCH, 8], F32)
    sb_f = A.alloc([NCH, 8], F32)
    td_i = A.alloc([NCH, 8], I32)
    rowb_i = A.alloc([8], I32)
    rec_raw = A.alloc([NCH * 16], F32)
    recf = rec_raw.rearrange("p (c k w) -> p c k w", c=NCH, k=8)
    reci = rec_raw.bitcast(I32).rearrange("p (c k w) -> p c k w", c=NCH, k=8)
    kj2 = A.alloc([NE], F32)
    tab_sb = A.alloc([NB, 2], I32)
    V("dve", lambda e: e.tensor_scalar(out=padf, in0=run_bc, scalar1=127.0, scalar2=None, op0=ALU.add), ["run_bc"], ["padf"])
    V("dve", lambda e: e.tensor_copy(padi, padf), ["padf"], ["padi"])
    V("dve", lambda e: e.tensor_scalar(out=padi, in0=padi, scalar1=7, scalar2=7, op0=ALU.arith_shift_right, op1=ALU.logical_shift_left), ["padi"], ["padi"])
    V("dve", lambda e: e.tensor_copy(padf, padi), ["padi"], ["padf"])
    V("dve", lambda e: e.memset(onesrow, 1.0), [], ["onesrow"])
    V("dve", lambda e: e.tensor_tensor_scan(out=pend, data0=onesrow, data1=padf, initial=0.0, op0=ALU.mult, op1=ALU.add), ["onesrow", "padf"], ["pend"])
    V("dve", lambda e: e.tensor_tensor(out=pstart, in0=pend, in1=padf, op=ALU.subtract), ["pend", "padf"], ["pstart"])
    for k in range(2):
        V("dve", lambda e, k=k: e.tensor_tensor(out=Dk[k], in0=ident_f, in1=pend[:, k * 128:(k + 1) * 128], op=ALU.mult), ["ident_f", "pend"], ["Dk%d" % k])
        mm(psf[0][:, k:k + 1], Dk[k], ones_f[:, 0:1], True, True, ["Dk%d" % k, "ones_f"], ["psf0"])
    V("dve", lambda e: e.tensor_copy(pcol2, psf[0][:, 0:2]), ["psf0"], ["pcol2"])
    V("pool", lambda e: e.iota(bvals_i, pattern=[[128, NB]], base=0, channel_multiplier=0), [], ["bvals_i"])
    V("dve", lambda e: e.tensor_copy(bvals, bvals_i), ["bvals_i"], ["bvals"])
    for k in range(2):
        V("dve", lambda e, k=k: e.tensor_scalar(out=ind[k], in0=bvals, scalar1=pcol2[:, k:k + 1], scalar2=None, op0=ALU.is_ge), ["bvals", "pcol2"], ["ind%d" % k])
        mm(psf[1][:, 0:NB], ones_b, ind[k], k == 0, k == 1, ["ones_b", "ind%d" % k], ["psf1"])
    V("dve", lambda e: e.tensor_scalar(out=widx_f, in0=psf[1][:, 0:NB], scalar1=128.0, scalar2=pcol[:, 0:1], op0=ALU.mult, op1=ALU.add), ["psf1", "pcol"], ["widx_f"])
    V("dve", lambda e: e.tensor_copy(widx_i, widx_f), ["widx_f"], ["widx_i"])
    V("pool", lambda e: e.iota(tabtmp, pattern=[[0, NB]], base=S * 8 + 128, channel_multiplier=0), [], ["tabtmp"])
    V("dve", lambda e: e.memset(tab0, 0), [], ["tab0"])
    V("dve", lambda e: e.tensor_copy(tab0[:, :, 0], tabtmp), ["tabtmp", "tab0"], ["tab0"])
    DMA("sp", lambda e: e.dma_start(out=tab_d.rearrange("(p b) w -> p (b w)", p=128), in_=tab0.rearrange("p b w -> p (b w)")), ["tab0"], ["tabinit"])
    V("dve", lambda e: e.memset(zrow, 0.0), [], ["zrow"])
    DMA("sp", lambda e: e.dma_start(out=h2tok_d[S:S + 16, :], in_=zrow[0:16, :]), ["zrow"], [("h2tokd", "z")])
    V("pool", lambda e: e.iota(rowb_i, pattern=[[1, 8]], base=0, channel_multiplier=8), [], ["rowb_i"])
    V("dve", lambda e: e.memset(pst8s, 0.0), [], ["pst8s"])
    for c in range(NCH):
        for k in range(8):
            V("dve", lambda e, c=c, k=k: e.scalar_tensor_tensor(out=kj2, in0=iota_e, scalar=e8s[:, c, k:k + 1], in1=pstart, op0=ALU.is_equal, op1=ALU.mult, accum_out=pst8s[:, c, k:k + 1]), ["iota_e", "e8s", "pstart", "pst8s", "kj2"], ["kj2", "pst8s"])
    V("dve", lambda e: e.tensor_tensor(out=slot_f, in0=pst8s, in1=pos8s, op=ALU.add), ["pst8s", "pos8s"], ["slot_f"])
    V("dve", lambda e: e.tensor_copy(slot_i, slot_f), ["slot_f"], ["slot_i"])
    V("dve", lambda e: e.tensor_scalar(out=sp_i, in0=slot_i, scalar1=127, scalar2=None, op0=ALU.bitwise_and), ["slot_i"], ["sp_i"])
    V("dve", lambda e: e.tensor_scalar(out=sb_i, in0=slot_i, scalar1=7, scalar2=None, op0=ALU.arith_shift_right), ["slot_i"], ["sb_i"])
    V("dve", lambda e: e.tensor_copy(sp_f, sp_i), ["sp_i"], ["sp_f"])
    V("dve", lambda e: e.tensor_copy(sb_f, sb_i), ["sb_i"], ["sb_f"])
    V("dve", lambda e: e.scalar_tensor_tensor(out=slot_f, in0=sp_f, scalar=float(NB), in1=sb_f, op0=ALU.mult, op1=ALU.add), ["sp_f", "sb_f", "slot_i"], ["slot_f"])
    V("dve", lambda e: e.tensor_copy(td_i, slot_f), ["slot_f"], ["td_i"])
    for c in range(NCH):
        V("dve", lambda e, c=c: e.tensor_scalar(out=reci[:, c, :, 0], in0=rowb_i, scalar1=c * 128 * 8, scalar2=None, op0=ALU.add), ["rowb_i"], ["rec"])
    V("dve", lambda e: e.tensor_copy(recf[:, :, :, 1], w8s), ["w8s", "rec"], ["rec"])
    SC_KEYS = []
    for c in range(NCH):
        for k in range(8):
            DMA("pool", lambda e, c=c, k=k: e.indirect_dma_start(out=tab_d, out_offset=IO(td_i[:, c, k:k + 1]), in_=reci[:, c, k, :], in_offset=None), ["rec", "td_i", "tabinit"], [("tabsc", c, k)])
            SC_KEYS.append(("tabsc", c, k))
    DMA("sp", lambda e: e.dma_start(out=tab_sb.rearrange("p b w -> p (b w)"), in_=tab_d.rearrange("(p b) w -> p (b w)", p=128)), SC_KEYS + ["tabinit"], ["tab_sb"])
    V("dve", lambda e: e.tensor_copy(ridx, tab_sb[:, :, 0]), ["tab_sb"], ["ridx"])
    V("dve", lambda e: e.tensor_scalar(out=gidx, in0=ridx, scalar1=3, scalar2=None, op0=ALU.arith_shift_right), ["ridx"], ["gidx"])
    V("dve", lambda e: e.tensor_copy(wcol_raw, tab_sb[:, :, 1]), ["tab_sb"], ["wcol"])
    barrier()
    A.off = MARK3
    NBUF = 3
    wall = [A.alloc([6144], BF16) for _ in range(NBUF)]
    wgb = [w[:, 0:2048] for w in wall]
    wub = [w[:, 2048:4096] for w in wall]
    wdb = [w[:, 4096:6144] for w in wall]
    Xe = [A.alloc([D], BF16) for _ in range(NBUF)]
    XeT = [A.alloc([8, 128], BF16) for _ in range(2)]
    hgb = [A.alloc([256], F32) for _ in range(2)]
    hidTb = [A.alloc([2, 128], BF16) for _ in range(2)]
    Ye = [A.alloc([D], F32) for _ in range(2)]
    H2TOK_KEYS = [("h2tokd", c) for c in range(NCH)] + [("h2tokd", "z")]
    WMAX = NE * 128 - 1
    bc_reg = {}

    def gathers(b):
        i = b % NBUF
        def wgather(e):
            if "r" not in bc_reg:
                bc_reg["r"] = e.to_reg(WMAX)
            return e.indirect_dma_start(out=wall[i], out_offset=None, in_=weL_d, in_offset=IO(widx_i[:, b:b + 1]), bounds_check=bc_reg["r"], oob_is_err=False)
        DMA("pool", wgather, ["widx_i"], ["wall%d" % i])
        def xgather(e):
            if "x" not in bc_reg:
                bc_reg["x"] = e.to_reg(S + 15)
            return e.indirect_dma_start(out=Xe[i], out_offset=None, in_=h2tok_d, in_offset=IO(gidx[:, b:b + 1]), bounds_check=bc_reg["x"], oob_is_err=False)
        DMA("pool", xgather, ["gidx"] + H2TOK_KEYS, ["Xe%d" % i])

    YKEYS = []
    for i_ in range(NBUF):
        V("dve", lambda e, i_=i_: e.memset(Xe[i_], 0.0), [], ["Xe%d" % i_])
    for b in range(min(NBUF, NB)):
        gathers(b)
    for b in range(NB):
        i = b % NBUF
        j = b % 2
        pXe = psb[j].rearrange("p (a b) -> p a b", a=8)
        for k in range(8):
            tr(pXe[:, k, :], Xe[i][:, k * 128:(k + 1) * 128], ident_b, ["Xe%d" % i, "ident_b"], ["psb%d" % j])
        V("dve" if j == 0 else "act", (lambda j: (lambda e: e.tensor_copy(XeT[j].rearrange("p a b -> p (a b)"), psb[j][:, :])) if j == 0 else (lambda e: e.copy(XeT[j].rearrange("p a b -> p (a b)"), psb[j][:, :])))(j), ["psb%d" % j], ["XeT%d" % j])
        pgu = psf[j]
        for m in range(4):
            wsrc_ = wgb[i] if m < 2 else wub[i]
            wk_ = "wall%d" % i
            for k in range(8):
                mm(pgu[:, m * 128:(m + 1) * 128], wsrc_[:, k * 256 + (m % 2) * 128:k * 256 + (m % 2 + 1) * 128], XeT[j][:, k, :], k == 0, k == 7, [wk_, "XeT%d" % j], ["psf%d" % j])
        V("act", lambda e, j=j, pgu=pgu: e.activation(out=hgb[j], in_=pgu[:, 0:256], func=AF.Silu), ["psf%d" % j], ["hg%d" % j])
        V("dve", lambda e, j=j, pgu=pgu: e.tensor_tensor(out=hidTb[j].rearrange("p a b -> p (a b)"), in0=pgu[:, 256:512], in1=hgb[j], op=ALU.mult), ["psf%d" % j, "hg%d" % j], ["hidT%d" % j])
        for nh in range(2):
            pd = psf[2 + 2 * j + nh]
            pdk = "psf%d" % (2 + 2 * j + nh)
            for kc in range(2):
                mm(pd[:, :], hidTb[j][:, kc, :], wdb[i][:, kc * 1024 + nh * 512:kc * 1024 + (nh + 1) * 512], kc == 0, kc == 1, ["hidT%d" % j, "wall%d" % i], [pdk])
            V("act", lambda e, j=j, nh=nh, pd=pd, b=b: e.activation(out=Ye[j][:, nh * 512:(nh + 1) * 512], in_=pd[:, :], func=AF.Copy, scale=wcol[:, b:b + 1]), [pdk, "wcol"], ["Ye%d" % j])
        if b + NBUF < NB:
            gathers(b + NBUF)
        def yscatter(e, j=j, b=b):
            if "y" not in bc_reg:
                bc_reg["y"] = e.to_reg(S * 8 + 127)
            return e.indirect_dma_start(out=yall_d, out_offset=IO(ridx[:, b:b + 1]), in_=Ye[j], in_offset=None, bounds_check=bc_reg["y"], oob_is_err=False)
        DMA("pool", yscatter, ["Ye%d" % j, "ridx"], [("yall", b)])
        YKEYS.append(("yall", b))
    barrier()
    A.off = MARK2
    TQ = min(S, 512)
    NQ = S // TQ
    NTL = TQ // 128
    NT = min(512, TQ)
    h2q = A.alloc([8, TQ], BF16)
    acc = A.alloc([NTL, D], F32)
    wg = A.alloc([8, 256], BF16)
    wu = A.alloc([8, 256], BF16)
    wdn = A.alloc([2, D], BF16)
    hg = A.alloc([2, NT], F32)
    hidT = A.alloc([2, NT], BF16)
    Yt = A.alloc([8, D], F32)
    x2t = A.alloc([D], F32)
    x3 = A.alloc([D], F32)
    ojunk = A.alloc([D], F32)
    fs = A.alloc([4], F32)
    fnw_bc = A.alloc([D], F32)
    DMA("sp", lambda e: e.dma_start(out=fnw_bc, in_=fnw_d.to_broadcast([128, D])), [], ["fnw_bc"])
    ALLH = [("h2Td", c) for c in range(NCH)]
    ALLX = [("x2d", c) for c in range(NCH)]
    DMA("pool", lambda e: e.dma_start(out=wg, in_=wsg_d.rearrange("(k p) n -> p k n", p=128)), [], ["wg"])
    DMA("pool", lambda e: e.dma_start(out=wu, in_=wsu_d.rearrange("(k p) n -> p k n", p=128)), [], ["wu"])
    DMA("pool", lambda e: e.dma_start(out=wdn, in_=wsd_d.rearrange("(k p) n -> p k n", p=128)), [], ["wdn"])
    for qd in range(NQ):
        q0 = qd * TQ
        DMA("sp", lambda e, q0=q0: e.dma_start(out=h2q, in_=h2T_d[:, :, q0:q0 + TQ]), ALLH, ["h2q"])
        for tt in range(0, TQ, NT):
            for m in range(4):
                wsrc_ = wg if m < 2 else wu
                wk_ = "wg" if m < 2 else "wu"
                for k in range(8):
                    mm(psf[m][:, 0:NT], wsrc_[:, k, (m % 2) * 128:(m % 2 + 1) * 128], h2q[:, k, tt:tt + NT], k == 0, k == 7, [wk_, "h2q"], ["psf%d" % m])
            for c in range(2):
                V("act", lambda e, c=c: e.activation(out=hg[:, c, :], in_=psf[c][:, 0:NT], func=AF.Silu), ["psf%d" % c], ["hg"])
                V("dve", lambda e, c=c: e.tensor_tensor(out=hidT[:, c, :], in0=psf[2 + c][:, 0:NT], in1=hg[:, c, :], op=ALU.mult), ["psf%d" % (2 + c), "hg"], ["hidT"])
            for ti in range(NT // 128):
                tile_i = tt // 128 + ti
                for nh in range(2):
                    for kc in range(2):
                        mm(psf[4 + nh][:, :], hidT[:, kc, ti * 128:(ti + 1) * 128], wdn[:, kc, nh * 512:(nh + 1) * 512], kc == 0, kc == 1, ["hidT", "wdn"], ["psf%d" % (4 + nh)])
                    V("act", lambda e, nh=nh, tile_i=tile_i: e.copy(acc[:, tile_i, nh * 512:(nh + 1) * 512], psf[4 + nh][:, :]), ["psf%d" % (4 + nh)], ["acc"])
        for ti in range(NTL):
            t0 = q0 + ti * 128
            DMA("sp", lambda e, t0=t0: e.dma_start(out=Yt, in_=yall_d[t0 * 8:(t0 + 128) * 8, :].rearrange("(p k) d -> p k d", k=8)), YKEYS, ["Yt"])
            DMA("sp", lambda e, t0=t0: e.dma_start(out=x2t, in_=x2_d[t0:t0 + 128, :]), ALLX, ["x2t"])
            V("pool", lambda e: e.tensor_tensor(out=Yt[:, 0:4, :], in0=Yt[:, 0:4, :], in1=Yt[:, 4:8, :], op=ALU.add), ["Yt"], ["Yt"])
            V("dve", lambda e: e.tensor_tensor(out=Yt[:, 0:2, :], in0=Yt[:, 0:2, :], in1=Yt[:, 2:4, :], op=ALU.add), ["Yt"], ["Yt"])
            V("dve", lambda e: e.tensor_tensor(out=Yt[:, 0, :], in0=Yt[:, 0, :], in1=Yt[:, 1, :], op=ALU.add), ["Yt"], ["Yt"])
            V("dve", lambda e, ti=ti: e.tensor_tensor(out=x3, in0=acc[:, ti, :], in1=Yt[:, 0, :], op=ALU.add), ["acc", "Yt"], ["x3"])
            V("dve", lambda e: e.tensor_tensor(out=x3, in0=x3, in1=g2_bc, op=ALU.mult), ["x3", "g2_bc"], ["x3"])
            V("dve", lambda e: e.tensor_tensor(out=x3, in0=x3, in1=x2t, op=ALU.add), ["x3", "x2t"], ["x3"])
            V("dve", lambda e: e.memset(fs, 0.0), [], ["fs"])
            V("act", lambda e: e.activation(out=ojunk, in_=x3, func=AF.Square, accum_out=fs[:, 0:1]), ["x3", "fs"], ["ojunk", "fs"])
            V("dve", lambda e: e.tensor_scalar(out=fs[:, 1:2], in0=fs[:, 0:1], scalar1=1.0 / D, scalar2=EPS, op0=ALU.mult, op1=ALU.add), ["fs"], ["fs"])
            V("act", lambda e: e.activation(out=fs[:, 3:4], in_=fs[:, 1:2], func=AF.Ln), ["fs"], ["fs"])
            V("act", lambda e: e.activation(out=fs[:, 2:3], in_=fs[:, 3:4], func=AF.Exp, scale=-0.5), ["fs"], ["fs"])
            V("dve", lambda e: e.scalar_tensor_tensor(out=ojunk, in0=x3, scalar=fs[:, 2:3], in1=fnw_bc, op0=ALU.mult, op1=ALU.mult), ["x3", "fs", "fnw_bc", "ojunk"], ["ojunk"])
            DMA("sp", lambda e, t0=t0: e.dma_start(out=out_d[t0:t0 + 128, :], in_=ojunk), ["ojunk"], [("outd", t0)])
    return locals()


def _host_inputs(inp, b, S):
    f = lambda a: np.ascontiguousarray(np.asarray(a, dtype=np.float32))
    col = lambda v: f(np.asarray(v).reshape(8, 128).T)
    weL = np.ascontiguousarray(np.concatenate([
        np.asarray(inp["w_exp_gate"][0], dtype=np.float32).reshape(256, 8, 128, 256).transpose(0, 2, 1, 3).reshape(256 * 128, 2048),
        np.asarray(inp["w_exp_up"][0], dtype=np.float32).reshape(256, 8, 128, 256).transpose(0, 2, 1, 3).reshape(256 * 128, 2048),
        np.asarray(inp["w_exp_down"][0], dtype=np.float32).reshape(256, 2, 128, 1024).transpose(0, 2, 1, 3).reshape(256 * 128, 2048)], axis=1))
    return {
        "x": f(inp["x"][b, :S]), "pos": np.ascontiguousarray(np.asarray(inp["positions"][b, :S], dtype=np.int32).reshape(1, S)),
        "ccol": col(inp["c"][b]), "w_ada": f(inp["w_ada"][0]), "b_ada": f(np.asarray(inp["b_ada"][0]).reshape(1, -1)),
        "n1col": col(inp["norm1_w"][0]), "n2col": col(inp["norm2_w"][0]), "w_in": f(inp["w_in"][0]),
        "convw": f(np.asarray(inp["conv_w"][0]).T.reshape(24, 128, 4).transpose(1, 0, 2)),
        "convb": f(np.asarray(inp["conv_b"][0]).reshape(24, 128).T),
        "dt_bias": f(np.asarray(inp["dt_bias"][0]).reshape(1, 32)), "a_log": f(np.asarray(inp["a_log"][0]).reshape(1, 32)),
        "d_skip": f(np.asarray(inp["d_skip"][0]).reshape(1, 32)),
        "ssm_norm_w": f(np.asarray(inp["ssm_norm_w"][0]).reshape(1, 2048)), "w_ssm_out": f(inp["w_ssm_out"][0]), "w_ret_out": f(inp["w_ret_out"][0]),
        "w_out": f(inp["w_out"][0]), "w_router": f(inp["w_router"][0]), "router_bias": f(np.asarray(inp["router_bias"][0]).reshape(1, 256)),
        "weL": weL,
        "w_sh_gate": f(inp["w_sh_gate"][0]), "w_sh_up": f(inp["w_sh_up"][0]), "w_sh_down": f(inp["w_sh_down"][0]),
        "final_norm_w": f(np.asarray(inp["final_norm_w"]).reshape(1, 1024)),
    }


def kernel(**inputs):
    B, S = inputs["x"].shape[0], inputs["x"].shape[1]
    nc = bass.Bass("TRN2", target_bir_lowering=False)
    L = build(nc, S)
    L["P"].emit(L["st"])
    L["st"].close()
    shared = _host_inputs(inputs, 0, S)
    in_maps = []
    for b in range(B):
        m = dict(shared)
        m["x"] = np.ascontiguousarray(np.asarray(inputs["x"][b], dtype=np.float32))
        m["pos"] = np.ascontiguousarray(np.asarray(inputs["positions"][b], dtype=np.int32).reshape(1, S))
        m["ccol"] = np.ascontiguousarray(np.asarray(inputs["c"][b], dtype=np.float32).reshape(8, 128).T)
        in_maps.append(m)
    res = run_bass_kernel_spmd(nc, in_maps, core_ids=list(range(B)))
    return np.stack([np.asarray(r["out"], dtype=np.float32) for r in res.results], axis=0)
```

```python
import numpy as np
from contextlib import ExitStack
from concourse.bass_utils import run_bass_kernel_spmd
import concourse.bass as bass
import concourse.mybir as mybir

F32 = mybir.dt.float32
BF16 = mybir.dt.bfloat16
I32 = mybir.dt.int32
U32 = mybir.dt.uint32
AF = mybir.ActivationFunctionType
ALU = mybir.AluOpType
AX = mybir.AxisListType


class _Op:
    __slots__ = ("eng", "fn", "reads", "writes", "is_dma", "deps", "sem", "val", "needs_inc", "idx", "pe_acc")

    def __init__(self, eng, fn, reads, writes, is_dma):
        self.eng = eng
        self.fn = fn
        self.reads = reads
        self.writes = writes
        self.is_dma = is_dma
        self.deps = []
        self.sem = None
        self.val = 0
        self.needs_inc = False


class Prog:
    ENGS = ("pe", "act", "dve", "pool", "sp")
    NDMA_SEM = {"sp": 6, "act": 2, "pool": 6}

    def __init__(self, nc):
        self.nc = nc
        self.ops = []
        self.last_writer = {}
        self.readers = {}
        self.alias = {}

    def _add(self, eng, fn, reads, writes, is_dma):
        reads = tuple(self.alias.get(k, k) if isinstance(k, str) else k for k in reads)
        writes = tuple(self.alias.get(k, k) if isinstance(k, str) else k for k in writes)
        op = _Op(eng, fn, reads, writes, is_dma)
        op.idx = len(self.ops)
        deps = set()
        for r in op.reads:
            w = self.last_writer.get(r)
            if w is not None:
                deps.add(w)
        for w_ in op.writes:
            w = self.last_writer.get(w_)
            if w is not None:
                deps.add(w)
            for rd in self.readers.get(w_, ()):
                deps.add(rd)
        deps.discard(op.idx)
        op.deps = sorted(deps)
        for r in op.reads:
            self.readers.setdefault(r, []).append(op.idx)
        for w_ in op.writes:
            self.last_writer[w_] = op.idx
            self.readers[w_] = []
        self.ops.append(op)
        return op

    def op(self, eng, fn, reads=(), writes=()):
        return self._add(eng, fn, reads, writes, False)

    def dma(self, queue, fn, reads=(), writes=()):
        return self._add(queue, fn, reads, writes, True)

    def barrier(self, touch):
        keys = list(set(self.last_writer.keys()) | set(self.readers.keys()))
        for e, (is_dma, fn) in touch.items():
            self._add(e, fn, (), keys + ["__bar"], is_dma)
        for e, (is_dma, fn) in touch.items():
            self._add(e, fn, ["__bar"], [("__bar2", e)], is_dma)

    def emit(self, stack):
        nc = self.nc
        ops = self.ops
        eng_sem = {}
        for e in ("pe", "act", "dve", "pool"):
            eng_sem[e] = stack.enter_context(nc.semaphore("s_" + e))
        dma_sems = {}
        for q, n in self.NDMA_SEM.items():
            dma_sems[q] = [stack.enter_context(nc.semaphore("d_%s%d" % (q, i))) for i in range(n)]
        dma_count = {q: 0 for q in self.NDMA_SEM}
        sem_last = {}
        sem_cnt = {}
        for o in ops:
            if o.is_dma:
                q = o.eng
                i = dma_count[q]
                dma_count[q] += 1
                s = dma_sems[q][i % len(dma_sems[q])]
                key = (q, i % len(dma_sems[q]))
                prev = sem_last.get(key)
                if prev is not None and prev not in o.deps:
                    o.deps.append(prev)
                sem_last[key] = o.idx
                sem_cnt[key] = sem_cnt.get(key, 0) + 1
                o.sem = s
                o.val = 16 * sem_cnt[key]
                o.needs_inc = True
        for o in ops:
            for d in o.deps:
                p = ops[d]
                if p.is_dma:
                    continue
                if p.eng == "pe" and o.eng == "pe" and not o.is_dma:
                    continue
                p.needs_inc = True
        cnt = {e: 0 for e in eng_sem}
        for o in ops:
            if not o.is_dma:
                o.sem = eng_sem[o.eng]
                if o.needs_inc:
                    cnt[o.eng] += 1
                o.val = cnt[o.eng]
        streams = {e: [] for e in self.ENGS}
        for o in ops:
            streams[o.eng].append(o)
        self.n_wait = 0

        def run(engname, eng):
            waited = {}
            for o in streams[engname]:
                need = {}
                for d in o.deps:
                    p = ops[d]
                    if (not p.is_dma) and p.eng == "pe" and engname == "pe" and not o.is_dma:
                        continue
                    k = id(p.sem)
                    if waited.get(k, 0) >= p.val:
                        continue
                    if k not in need or need[k][1] < p.val:
                        need[k] = (p.sem, p.val)
                for k, (s, v) in need.items():
                    eng.wait_ge(s, v)
                    waited[k] = v
                    self.n_wait += 1
                ins = o.fn(eng)
                if o.needs_inc:
                    ins.then_inc(o.sem, 16 if o.is_dma else 1)
            if engname in dma_sems:
                for i, s in enumerate(dma_sems[engname]):
                    key = (engname, i)
                    if key in sem_cnt and waited.get(id(s), 0) < 16 * sem_cnt[key]:
                        eng.wait_ge(s, 16 * sem_cnt[key])

        block = stack.enter_context(nc.Block())

        @block.tensor
        def _(e):
            run("pe", e)

        @block.scalar
        def _(e):
            run("act", e)

        @block.vector
        def _(e):
            run("dve", e)

        @block.gpsimd
        def _(e):
            run("pool", e)

        @block.sync
        def _(e):
            run("sp", e)


import math

D = 1024
NH_S = 32
HP = 64
NST = 128
NG = 4
IN_DIM = 13344
OFF_Z, OFF_XBC, OFF_DT, OFF_Q, OFF_K, OFF_V, OFF_G, OFF_GS, OFF_GR = 0, 2048, 5120, 5152, 6176, 7200, 9248, 11296, 12320
EPS = 1e-6
NE = 256


class Arena:
    def __init__(self, t, nbytes):
        self.t = t
        self.nbytes = nbytes
        self.off = 0

    def alloc(self, free_shape, dt):
        esz = 2 if dt == BF16 else 4
        n = 1
        for s in free_shape:
            n *= s
        nb = n * esz
        self.off = (self.off + 63) // 64 * 64
        assert self.off + nb <= self.nbytes, ("arena overflow", self.off, nb)
        v = self.t[:, self.off // 2:(self.off + nb) // 2]
        self.off += nb
        if dt != BF16:
            v = v.bitcast(dt)
        if len(free_shape) == 2:
            v = v.rearrange("p (a b) -> p a b", a=free_shape[0])
        elif len(free_shape) == 3:
            v = v.rearrange("p (a b c) -> p a b c", a=free_shape[0], b=free_shape[1])
        return v


def build(nc, S, dbg=False, stop=99):
    NCH = S // 128
    st = ExitStack()
    P = Prog(nc)
    dram = lambda name, shape, dt, kind="ExternalInput": nc.dram_tensor(name, list(shape), dt, kind=kind).ap()
    x_d = dram("x", [S, D], F32)
    pos_d = dram("pos", [1, S], I32)
    ccol_d = dram("ccol", [128, 8], F32)
    wada_d = dram("w_ada", [D, 6 * D], F32)
    bada_d = dram("b_ada", [1, 6 * D], F32)
    n1_d = dram("n1col", [128, 8], F32)
    n2_d = dram("n2col", [128, 8], F32)
    win_d = dram("w_in", [D, IN_DIM], F32)
    convw_d = dram("convw", [128, 24, 4], F32)
    convb_d = dram("convb", [128, 24], F32)
    dtb_d = dram("dt_bias", [1, 32], F32)
    alog_d = dram("a_log", [1, 32], F32)
    dsk_d = dram("d_skip", [1, 32], F32)
    snw_d = dram("ssm_norm_w", [1, 2048], F32)
    wso_d = dram("w_ssm_out", [2048, D], F32)
    wro_d = dram("w_ret_out", [2048, D], F32)
    wo_d = dram("w_out", [D, D], F32)
    wr_d = dram("w_router", [D, NE], F32)
    rb_d = dram("router_bias", [1, NE], F32)
    wsg_d = dram("w_sh_gate", [D, 256], F32)
    wsu_d = dram("w_sh_up", [D, 256], F32)
    wsd_d = dram("w_sh_down", [256, D], F32)
    fnw_d = dram("final_norm_w", [1, D], F32)
    out_d = dram("out", [S, D], F32, kind="ExternalOutput")
    winb_d = dram("winb", [D, IN_DIM], BF16, kind="Internal")
    wsob_d = dram("wsob", [2048, D], BF16, kind="Internal")
    wrob_d = dram("wrob", [2048, D], BF16, kind="Internal")
    wob_d = dram("wob", [D, D], BF16, kind="Internal")
    x2_d = dram("x2s", [S, D], F32, kind="Internal")
    h2T_d = dram("h2Ts", [128, 8, S], BF16, kind="Internal")
    weL_d = dram("weL", [NE * 128, 6144], F32)
    NB = S * 8 // 128 + NE
    h2tok_d = dram("h2tok", [S + 16, D], BF16, kind="Internal")
    tab_d = dram("tab", [128 * NB, 2], I32, kind="Internal")
    yall_d = dram("yall", [S * 8 + 128, D], F32, kind="Internal")
    shd_d = dram("shd", [S, D], F32, kind="Internal")
    dbg_d = {}

    ARB = 206 * 1024
    big = st.enter_context(nc.sbuf_tensor("big", [128, ARB // 2], BF16))
    A = Arena(big, ARB)
    psf = [st.enter_context(nc.psum_tensor("psf%d" % i, [128, 512], F32)) for i in range(6)]
    psb = [st.enter_context(nc.psum_tensor("psb%d" % i, [128, 1024], BF16)) for i in range(2)]

    V = lambda eng, fn, r, w: P.op(eng, fn, reads=r, writes=w)
    DMA = lambda q, fn, r, w: P.dma(q, fn, reads=r, writes=w)

    def mm(out, lhsT, rhs, start, stop, r, w):
        P.op("pe", lambda e: e.matmul(out, lhsT=lhsT, rhs=rhs, start=start, stop=stop), reads=r, writes=w)

    def tr(out, in_, ident, r, w):
        P.op("pe", lambda e: e.transpose(out, in_, ident), reads=r, writes=w)

    idi = A.alloc([128], I32)
    idf = A.alloc([128], F32)
    ident_b = A.alloc([128], BF16)
    ident_f = A.alloc([128], F32)
    tri_f = A.alloc([128], F32)
    ones_f = A.alloc([128], F32)
    negm8 = A.alloc([8, 128], BF16)
    idecT = A.alloc([4, 128], F32)
    qdec_bc = A.alloc([4, 128], F32)
    kdec = A.alloc([4], F32)
    pcol_i = A.alloc([1], I32)
    pcol = A.alloc([1], F32)
    invf = A.alloc([1], F32)
    V("pool", lambda e: e.iota(idi, pattern=[[1, 128]], base=0, channel_multiplier=-1), [], ["idi"])
    V("dve", lambda e: e.tensor_copy(idf, idi), ["idi"], ["idf"])
    V("dve", lambda e: e.tensor_single_scalar(ident_b, idf, 0.0, op=ALU.is_equal), ["idf"], ["ident_b"])
    V("dve", lambda e: e.tensor_single_scalar(ident_f, idf, 0.0, op=ALU.is_equal), ["idf"], ["ident_f"])
    V("dve", lambda e: e.tensor_single_scalar(tri_f, idf, 0.0, op=ALU.is_ge), ["idf"], ["tri_f"])
    V("dve", lambda e: e.memset(ones_f, 1.0), [], ["ones_f"])
    for i in range(8):
        V("dve", (lambda i: lambda e: e.tensor_scalar(out=negm8[:, i, :], in0=idf, scalar1=0.0, scalar2=-30000.0, op0=ALU.is_lt, op1=ALU.mult))(i), ["idf"], ["negm8"])
    V("pool", lambda e: e.iota(pcol_i, pattern=[[0, 1]], base=0, channel_multiplier=1), [], ["pcol_i"])
    V("dve", lambda e: e.tensor_copy(pcol, pcol_i), ["pcol_i"], ["pcol"])
    V("act", lambda e: e.activation(out=invf, in_=pcol, func=AF.Exp, scale=-math.log(10000.0) / 128.0), ["pcol"], ["invf"])
    lg = [math.log1p(-(2.0 ** (-5.0 - h))) for h in range(4)]
    tmpc = A.alloc([128], F32)
    for h in range(4):
        V("dve", lambda e: e.tensor_scalar(out=tmpc, in0=idf, scalar1=0.0, scalar2=None, op0=ALU.max), ["idf", "tmpc"], ["tmpc"])
        V("act", (lambda h: lambda e: e.activation(out=idecT[:, h, :], in_=tmpc, func=AF.Exp, scale=lg[h]))(h), ["tmpc"], ["idecT"])
        V("dve", (lambda h: lambda e: e.tensor_tensor(out=idecT[:, h, :], in0=idecT[:, h, :], in1=tri_f, op=ALU.mult))(h), ["idecT", "tri_f"], ["idecT"])
        V("dve", lambda e: e.tensor_scalar(out=tmpc, in0=idf, scalar1=pcol[:, 0:1], scalar2=1.0, op0=ALU.add, op1=ALU.add), ["idf", "pcol", "tmpc"], ["tmpc"])
        V("act", (lambda h: lambda e: e.activation(out=qdec_bc[:, h, :], in_=tmpc, func=AF.Exp, scale=lg[h]))(h), ["tmpc"], ["qdec_bc"])
        V("dve", lambda e: e.tensor_scalar(out=tmpc[:, 0:1], in0=pcol, scalar1=-1.0, scalar2=127.0, op0=ALU.mult, op1=ALU.add), ["pcol", "tmpc"], ["tmpc"])
        V("act", (lambda h: lambda e: e.activation(out=kdec[:, h:h + 1], in_=tmpc[:, 0:1], func=AF.Exp, scale=lg[h]))(h), ["tmpc"], ["kdec"])
    cdec = [math.exp(128.0 * lg[h]) for h in range(4)]

    ccol = A.alloc([8], F32)
    n1col = A.alloc([8], F32)
    n2col = A.alloc([8], F32)
    convw = A.alloc([24, 4], F32)
    convb = A.alloc([24], F32)
    dtb_bc = A.alloc([32], F32)
    A_bc = A.alloc([32], F32)
    D_bc = A.alloc([32], F32)
    snw_bc = A.alloc([2048], F32)
    rb_bc = A.alloc([NE], F32)
    for dst, src, k in ((ccol, ccol_d, "ccol"), (n1col, n1_d, "n1col"), (n2col, n2_d, "n2col"), (convw, convw_d, "convw"), (convb, convb_d, "convb")):
        DMA("sp", (lambda dst, src: lambda e: e.dma_start(out=dst, in_=src))(dst, src), [], [k])
    for dst, src, k, n in ((dtb_bc, dtb_d, "dtb_bc", 32), (A_bc, alog_d, "A_bc", 32), (D_bc, dsk_d, "D_bc", 32), (snw_bc, snw_d, "snw_bc", 2048),
                           (rb_bc, rb_d, "rb_bc", NE)):
        DMA("sp", (lambda dst, src, n: lambda e: e.dma_start(out=dst, in_=src.to_broadcast([128, n])))(dst, src, n), [], [k])
    V("act", lambda e: e.activation(out=A_bc, in_=A_bc, func=AF.Exp), ["A_bc"], ["A_bc"])
    V("dve", lambda e: e.tensor_scalar(out=A_bc, in0=A_bc, scalar1=-1.0, scalar2=None, op0=ALU.mult), ["A_bc"], ["A_bc"])

    for r0 in range(0, D, 128):
        DMA("pool", (lambda r0: lambda e: e.dma_start(out=winb_d[r0:r0 + 128, :], in_=win_d[r0:r0 + 128, :]))(r0), [], [("winb", r0)])
        DMA("pool", (lambda r0: lambda e: e.dma_start(out=wob_d[r0:r0 + 128, :], in_=wo_d[r0:r0 + 128, :]))(r0), [], [("wob", r0)])
    for r0 in range(0, 2048, 128):
        DMA("pool", (lambda r0: lambda e: e.dma_start(out=wsob_d[r0:r0 + 128, :], in_=wso_d[r0:r0 + 128, :]))(r0), [], [("wsob", r0)])
        DMA("pool", (lambda r0: lambda e: e.dma_start(out=wrob_d[r0:r0 + 128, :], in_=wro_d[r0:r0 + 128, :]))(r0), [], [("wrob", r0)])
    WINB_KEYS = [("winb", r0) for r0 in range(0, D, 128)]
    WOB_KEYS = [("wob", r0) for r0 in range(0, D, 128)]
    WSOB_KEYS = [("wsob", r0) for r0 in range(0, 2048, 128)]
    WROB_KEYS = [("wrob", r0) for r0 in range(0, 2048, 128)]

    if stop == 0:
        return locals()
    sc_col = A.alloc([8], F32)
    cols = A.alloc([32], F32)
    s1col = A.alloc([8], F32)
    sh1col = A.alloc([8], F32)
    s2col = A.alloc([8], F32)
    sh2col = A.alloc([8], F32)
    g1_bc = A.alloc([D], F32)
    g2_bc = A.alloc([D], F32)
    S_f = A.alloc([2048], F32)
    S_b = A.alloc([2048], BF16)
    R_f = A.alloc([8, 512], F32)
    R_b = A.alloc([8, 512], BF16)
    u = A.alloc([24, 131], F32)
    hT = A.alloc([8, 128], BF16)
    ynT = A.alloc([16, 128], BF16)
    yrT = A.alloc([16, 128], BF16)
    V("dve", lambda e: e.memset(S_f, 0.0), [], ["S_f"])
    V("dve", lambda e: e.memset(S_b, 0.0), [], ["S_b"])
    V("pool", lambda e: e.memset(R_f, 0.0), [], ["R_f"])
    V("pool", lambda e: e.memset(R_b, 0.0), [], ["R_b"])
    V("pool", lambda e: e.memset(u, 0.0), [], ["u"])
    striu_b = A.alloc([128], BF16)
    ones_b = A.alloc([128], BF16)
    iota_ei = A.alloc([NE], I32)
    iota_e = A.alloc([NE], F32)
    iota8k = A.alloc([NE], F32)
    run_bc = A.alloc([NE], F32)
    e8s = A.alloc([NCH, 8], F32)
    pos8s = A.alloc([NCH, 8], F32)
    w8s = A.alloc([NCH, 8], F32)
    V("dve", lambda e: e.tensor_single_scalar(striu_b, idf, 0.0, op=ALU.is_gt), ["idf"], ["striu_b"])
    V("dve", lambda e: e.memset(ones_b, 1.0), [], ["ones_b"])
    V("pool", lambda e: e.iota(iota_ei, pattern=[[1, NE]], base=0, channel_multiplier=0), [], ["iota_ei"])
    V("dve", lambda e: e.tensor_copy(iota_e, iota_ei), ["iota_ei"], ["iota_e"])
    V("dve", lambda e: e.tensor_scalar(out=iota8k, in0=iota_e, scalar1=8192.0, scalar2=None, op0=ALU.mult), ["iota_e"], ["iota8k"])
    V("dve", lambda e: e.memset(run_bc, 0.0), [], ["run_bc"])
    V("dve", lambda e: e.memset(w8s, 0.0), [], ["w8s"])
    MARK = A.off
    mod_row = A.alloc([6 * D], F32)
    bada = A.alloc([6 * D], F32)
    wa_buf = [A.alloc([8, 512], F32) for _ in range(2)]
    V("act", lambda e: e.activation(out=sc_col, in_=ccol, func=AF.Silu), ["ccol"], ["sc_col"])
    DMA("sp", lambda e: e.dma_start(out=bada[0:1, :], in_=bada_d), [], ["bada"])
    for nt in range(12):
        wb = wa_buf[nt % 2]
        kb = "wa%d" % (nt % 2)
        DMA("sp", (lambda wb, nt: lambda e: e.dma_start(out=wb, in_=wada_d[:, nt * 512:(nt + 1) * 512].rearrange("(k p) n -> p k n", p=128)))(wb, nt), [], [kb])
        for k in range(8):
            mm(psf[0][0:1, :], sc_col[:, k:k + 1], wb[:, k, :], k == 0, k == 7, [kb, "sc_col"], ["psf0"])
        V("dve", (lambda nt: lambda e: e.tensor_tensor(out=mod_row[0:1, nt * 512:(nt + 1) * 512], in0=psf[0][0:1, :], in1=bada[0:1, nt * 512:(nt + 1) * 512], op=ALU.add))(nt), ["psf0", "bada"], ["mod_row"])
    for qi, q in enumerate((0, 1, 3, 4)):
        for k in range(8):
            mm(psf[1][:, qi * 8 + k:qi * 8 + k + 1], mod_row[0:1, q * D + k * 128:q * D + (k + 1) * 128], ones_f[0:1, 0:1], True, True, ["mod_row", "ones_f"], ["psf1"])
    V("dve", lambda e: e.tensor_copy(cols, psf[1][:, 0:32]), ["psf1"], ["cols"])
    V("dve", lambda e: e.tensor_copy(sh1col, cols[:, 0:8]), ["cols"], ["sh1col"])
    V("dve", lambda e: e.scalar_tensor_tensor(out=s1col, in0=cols[:, 8:16], scalar=1.0, in1=n1col, op0=ALU.add, op1=ALU.mult), ["cols", "n1col"], ["s1col"])
    V("dve", lambda e: e.tensor_copy(sh2col, cols[:, 16:24]), ["cols"], ["sh2col"])
    V("dve", lambda e: e.scalar_tensor_tensor(out=s2col, in0=cols[:, 24:32], scalar=1.0, in1=n2col, op0=ALU.add, op1=ALU.mult), ["cols", "n2col"], ["s2col"])
    for gi, (gb, q, kk) in enumerate(((g1_bc, 2, "g1_bc"), (g2_bc, 5, "g2_bc"))):
        for hh in range(2):
            pp = psf[2 + hh]
            mm(pp[:, :], ones_f[0:1, :], mod_row[0:1, q * D + hh * 512:q * D + (hh + 1) * 512], True, True, ["mod_row", "ones_f"], ["psf%d" % (2 + hh)])
            V("dve", (lambda gb, hh, pp: lambda e: e.tensor_copy(gb[:, hh * 512:(hh + 1) * 512], pp[:, :]))(gb, hh, pp), ["psf%d" % (2 + hh)], [kk])

    if stop == 1:
        return locals()
    bscr = A.alloc([4], F32)
    bdr = dram("bdr", [1, 8], F32, kind="Internal")

    def barrier():
        P.barrier({
            "dve": (False, lambda e: e.memset(bscr[0:1, 0:1], 0.0)),
            "pool": (False, lambda e: e.memset(bscr[0:1, 1:2], 0.0)),
            "act": (False, lambda e: e.activation(out=bscr[0:1, 2:3], in_=ones_f[0:1, 0:1], func=AF.Copy)),
            "sp": (True, lambda e: e.dma_start(out=bdr[0:1, 0:4], in_=bdr[0:1, 4:8])),
        })

    barrier()
    A.off = MARK
    MARK2 = MARK

    NBW, LOOK = 4, 2
    wt = [A.alloc([8, 512], BF16) for _ in range(NBW)]
    SEQ = []
    for t0 in range(0, 2048, 512):
        SEQ.append(("winb", 0, OFF_Z + t0, 512))
    for t0 in range(0, 3072, 512):
        SEQ.append(("winb", 0, OFF_XBC + t0, 512))
    SEQ.append(("winb", 0, OFF_DT, 32))
    for off, n in ((OFF_Q, 1024), (OFF_K, 1024), (OFF_V, 2048), (OFF_G, 2048), (OFF_GS, 1024), (OFF_GR, 1024)):
        for t0 in range(0, n, 512):
            SEQ.append(("winb", 0, off + t0, 512))
    for nm in ("wsob", "wrob"):
        for nh in range(2):
            for kg in range(2):
                SEQ.append((nm, kg * 1024, nh * 512, 512))
    for nh in range(2):
        SEQ.append(("wob", 0, nh * 512, 512))
    WSRC = {"winb": (winb_d, WINB_KEYS), "wsob": (wsob_d, WSOB_KEYS), "wrob": (wrob_d, WROB_KEYS), "wob": (wob_d, WOB_KEYS)}
    ws_pos = [0]
    ws_issued = [0]
    WS_TOTAL = NCH * len(SEQ)

    def _issue_w(idx):
        nm, r0, c0, ncols = SEQ[idx % len(SEQ)]
        src_d, srckeys = WSRC[nm]
        i = idx % NBW
        b = wt[i]
        DMA("sp", lambda e: e.dma_start(out=b[:, :, 0:ncols], in_=src_d[r0:r0 + 1024, c0:c0 + ncols].rearrange("(k p) n -> p k n", p=128)), srckeys, ["wt%d" % i])

    def load_w(src_d, srckeys, r0, c0, ncols):
        j = ws_pos[0]
        nm, r0_, c0_, n_ = SEQ[j % len(SEQ)]
        assert WSRC[nm][0] is src_d and (r0_, c0_, n_) == (r0, c0, ncols), ("weight stream order mismatch", j, SEQ[j % len(SEQ)], r0, c0, ncols)
        while ws_issued[0] <= min(j + LOOK, WS_TOTAL - 1):
            _issue_w(ws_issued[0])
            ws_issued[0] += 1
        ws_pos[0] += 1
        return wt[j % NBW], "wt%d" % (j % NBW)

    pj_i = [0]

    def next_pj():
        i = pj_i[0] % 3
        pj_i[0] += 1
        return psf[i], "psf%d" % i

    def proj_tok(c0, ncols, evac):
        for t0 in range(0, ncols, 512):
            n = min(512, ncols - t0)
            b, bk = load_w(winb_d, WINB_KEYS, 0, c0 + t0, n)
            pp, pk = next_pj()
            for k in range(8):
                mm(pp[:, 0:n], hT[:, k, :], b[:, k, 0:n], k == 0, k == 7, ["hT", bk], [pk])
            evac(pp, pk, t0, n)

    def proj_feat(c0, ncols, evac):
        for t0 in range(0, ncols, 512):
            b, bk = load_w(winb_d, WINB_KEYS, 0, c0 + t0, 512)
            pp, pk = next_pj()
            for j in range(4):
                for k in range(8):
                    mm(pp[:, j * 128:(j + 1) * 128], b[:, k, j * 128:(j + 1) * 128], hT[:, k, :], k == 0, k == 7, ["hT", bk], [pk])
            evac(pp, pk, t0 // 128)

    qraw = A.alloc([8, 128], F32)
    kraw = A.alloc([8, 128], F32)
    vtok = A.alloc([2048], BF16)
    gsil = A.alloc([2048], BF16)
    sgT = A.alloc([8, 128], BF16)
    srT = A.alloc([8, 128], BF16)
    SCR0 = A.off
    for ch in range(NCH):
        A.off = SCR0
        tok0 = ch * 128
        xt = A.alloc([D], F32)
        xn = A.alloc([D], BF16)
        junk = A.alloc([D], BF16)
        ss = A.alloc([4], F32)
        DMA("sp", lambda e, tok0=tok0, xt=xt: e.dma_start(out=xt, in_=x_d[tok0:tok0 + 128, :]), [], ["xt"])
        V("dve", lambda e, ss=ss: e.memset(ss, 0.0), [], ["ss"])
        V("act", lambda e, xt=xt, junk=junk, ss=ss: e.activation(out=junk, in_=xt, func=AF.Square, accum_out=ss[:, 0:1]), ["xt", "ss"], ["junk", "ss"])
        V("dve", lambda e, ss=ss: e.tensor_scalar(out=ss[:, 1:2], in0=ss[:, 0:1], scalar1=1.0 / D, scalar2=EPS, op0=ALU.mult, op1=ALU.add), ["ss"], ["ss"])
        V("act", lambda e, ss=ss: e.activation(out=ss[:, 3:4], in_=ss[:, 1:2], func=AF.Ln), ["ss"], ["ss"])
        V("act", lambda e, ss=ss: e.activation(out=ss[:, 2:3], in_=ss[:, 3:4], func=AF.Exp, scale=-0.5), ["ss"], ["ss"])
        V("dve", lambda e, xt=xt, xn=xn, ss=ss: e.tensor_scalar(out=xn, in0=xt, scalar1=ss[:, 2:3], scalar2=None, op0=ALU.mult), ["xt", "ss"], ["xn"])
        pT = psb[0].rearrange("p (a b) -> p a b", a=8)
        for k in range(8):
            tr(pT[:, k, :], xn[:, k * 128:(k + 1) * 128], ident_b, ["xn", "ident_b"], ["psb0"])
        for k in range(8):
            V("act", lambda e, k=k, pT=pT: e.activation(out=hT[:, k, :], in_=pT[:, k, :], func=AF.Identity, bias=sh1col[:, k:k + 1], scale=s1col[:, k:k + 1]), ["psb0", "s1col", "sh1col"], ["hT"])

        if stop == 2:
            return locals()
        zs = A.alloc([2048], BF16)
        xc = A.alloc([24, 128], BF16)
        cacc = A.alloc([4, 128], F32)
        sm = A.alloc([8, 32], F32)
        xtok = A.alloc([2048], BF16)
        xdt = A.alloc([2048], BF16)
        Btok = A.alloc([512], BF16)
        Atri = A.alloc([8, 128], F32)
        dec = A.alloc([8, 128], BF16)
        wTt = A.alloc([8, 128], BF16)
        cbs = A.alloc([128], F32)
        t1 = A.alloc([512], F32)
        t2 = A.alloc([512], F32)
        yg = A.alloc([512], F32)
        yn = A.alloc([2048], BF16)
        gsm = A.alloc([16], F32)
        eal = A.alloc([8], F32)

        def ev_z(pp, pk, t0, n):
            V("act", lambda e: e.activation(out=zs[:, t0:t0 + n], in_=pp[:, 0:n], func=AF.Silu), [pk], ["zs"])
        proj_tok(OFF_Z, 2048, ev_z)

        def ev_xbc(pp, pk, j0):
            V("act", lambda e: e.copy(u[:, j0:j0 + 4, 3:131], pp[:, :].rearrange("p (a b) -> p a b", a=4)), [pk], ["u"])
        proj_feat(OFF_XBC, 3072, ev_xbc)

        def ev_dt(pp, pk, t0, n):
            V("dve", lambda e: e.tensor_tensor(out=sm[:, 0, :], in0=pp[:, 0:32], in1=dtb_bc, op=ALU.add), [pk, "dtb_bc"], ["sm"])
        proj_tok(OFF_DT, 32, ev_dt)
        V("act", lambda e: e.activation(out=sm[:, 6, :], in_=sm[:, 0, :], func=AF.Exp), ["sm"], ["sm"])
        V("act", lambda e: e.activation(out=sm[:, 1, :], in_=sm[:, 6, :], func=AF.Ln, bias=1.0), ["sm"], ["sm"])
        V("dve", lambda e: e.tensor_tensor(out=sm[:, 2, :], in0=sm[:, 1, :], in1=A_bc, op=ALU.mult), ["sm", "A_bc"], ["sm"])
        for j in range(24):
            cj = cacc[:, j % 4, :]
            ck = ("cacc", j % 4)
            V("dve", lambda e, j=j, cj=cj: e.tensor_scalar(out=cj, in0=u[:, j, 0:128], scalar1=convw[:, j, 0:1], scalar2=convb[:, j:j + 1], op0=ALU.mult, op1=ALU.add), ["u", "convw", "convb"], [ck])
            for t in range(1, 4):
                V("dve", lambda e, j=j, t=t, cj=cj: e.scalar_tensor_tensor(out=cj, in0=u[:, j, t:t + 128], scalar=convw[:, j, t:t + 1], in1=cj, op0=ALU.mult, op1=ALU.add), ["u", "convw", ck], [ck])
            V("act", lambda e, j=j, cj=cj: e.activation(out=xc[:, j, :], in_=cj, func=AF.Silu), [ck], ["xc"])
        V("pool", lambda e: e.tensor_copy(u[:, :, 0:3], u[:, :, 128:131]), ["u"], ["u"])
        if stop == 3:
            return locals()
        pX0 = psb[0].rearrange("p (a b) -> p a b", a=8)
        pX1 = psb[1].rearrange("p (a b) -> p a b", a=8)
        for j in range(8):
            tr(pX0[:, j, :], xc[:, j, :], ident_b, ["xc", "ident_b"], ["psb0"])
        for j in range(8):
            tr(pX1[:, j, :], xc[:, 8 + j, :], ident_b, ["xc", "ident_b"], ["psb1"])
        V("act", lambda e: e.copy(xtok[:, 0:1024], psb[0][:, :]), ["psb0"], ["xtok"])
        V("act", lambda e: e.copy(xtok[:, 1024:2048], psb[1][:, :]), ["psb1"], ["xtok"])
        if stop == 33:
            return locals()
        dtv_b = lambda h0, nh: sm[:, 1, h0:h0 + nh].unsqueeze(2).to_broadcast([128, nh, 64])
        V("dve", lambda e: e.tensor_tensor(out=xdt[:, 0:1024].rearrange("p (a b) -> p a b", a=16), in0=xtok[:, 0:1024].rearrange("p (a b) -> p a b", a=16), in1=dtv_b(0, 16), op=ALU.mult), ["xtok", "sm"], ["xdt"])
        V("dve", lambda e: e.tensor_tensor(out=xdt[:, 1024:2048].rearrange("p (a b) -> p a b", a=16), in0=xtok[:, 1024:2048].rearrange("p (a b) -> p a b", a=16), in1=dtv_b(16, 16), op=ALU.mult), ["xtok", "sm"], ["xdt"])
        for j in range(4):
            tr(pX0[:, j, :], xc[:, 16 + j, :], ident_b, ["xc", "ident_b"], ["psb0"])
        V("act", lambda e: e.copy(Btok, psb[0][:, 0:512]), ["psb0"], ["Btok"])
        if stop == 35:
            return locals()
        mm(psf[3][:, 0:32], tri_f, sm[:, 2, :], True, True, ["tri_f", "sm"], ["psf3"])
        V("dve", lambda e: e.tensor_copy(sm[:, 3, :], psf[3][:, 0:32]), ["psf3"], ["sm"])
        V("dve", lambda e: e.tensor_scalar(out=sm[:, 4, :], in0=sm[:, 3, :], scalar1=-1.0, scalar2=None, op0=ALU.mult), ["sm"], ["sm"])
        V("act", lambda e: e.activation(out=sm[:, 5, :], in_=sm[:, 3, :], func=AF.Exp), ["sm"], ["sm"])
        if stop == 4:
            return locals()
        for g in range(4):
            h0 = g * 8
            V("pool", lambda e, h0=h0: e.tensor_tensor(out=Atri, in0=tri_f.unsqueeze(1).to_broadcast([128, 8, 128]), in1=sm[:, 2, h0:h0 + 8].unsqueeze(2).to_broadcast([128, 8, 128]), op=ALU.mult), ["tri_f", "sm"], ["Atri"])
            segp = (psf[3], psf[4])
            for hh in range(2):
                mm(segp[hh][:, :], ones_f, Atri[:, hh * 4:(hh + 1) * 4, :].rearrange("p a b -> p (a b)"), True, False, ["ones_f", "Atri"], ["psf%d" % (3 + hh)])
                mm(segp[hh][:, :], ident_b, negm8[:, 0:4, :].rearrange("p a b -> p (a b)"), False, True, ["ident_b", "negm8"], ["psf%d" % (3 + hh)])
            if stop == 5:
                return locals()
            mm(psf[5][:, 0:128], xc[:, 16 + g, :], xc[:, 20 + g, :], True, True, ["xc"], ["psf5"])
            V("dve", lambda e: e.tensor_copy(cbs, psf[5][:, 0:128]), ["psf5"], ["cbs"])
            for hl in range(8):
                sp_ = segp[hl // 4]
                V("act", lambda e, hl=hl, sp_=sp_, h0=h0: e.activation(out=dec[:, hl, :], in_=sp_[:, (hl % 4) * 128:(hl % 4 + 1) * 128], func=AF.Exp, bias=sm[:, 4, h0 + hl:h0 + hl + 1], scale=1.0), ["psf%d" % (3 + hl // 4), "sm"], ["dec"])
            for hh in range(2):
                V("act", lambda e, hh=hh: e.activation(out=eal[:, hh * 4:(hh + 1) * 4], in_=segp[hh][:, :].rearrange("p (a b) -> p a b", a=4)[:, :, 127], func=AF.Exp), ["psf%d" % (3 + hh)], ["eal"])
                V("dve", lambda e, hh=hh, h0=h0: e.tensor_tensor(out=sm[:, 6, h0 + hh * 4:h0 + hh * 4 + 4], in0=segp[hh][:, :].rearrange("p (a b) -> p a b", a=4)[:, :, 127], in1=sm[:, 4, h0 + hh * 4:h0 + hh * 4 + 4], op=ALU.add), ["psf%d" % (3 + hh), "sm"], ["sm"])
            V("act", lambda e, h0=h0: e.activation(out=sm[:, 7, h0:h0 + 8], in_=sm[:, 6, h0:h0 + 8], func=AF.Exp), ["sm"], ["sm"])
            V("dve", lambda e: e.tensor_tensor(out=wTt, in0=dec, in1=cbs.unsqueeze(1).to_broadcast([128, 8, 128]), op=ALU.mult), ["dec", "cbs"], ["wTt"])
            for hl in range(8):
                mm(psf[5][:, hl * 64:(hl + 1) * 64], wTt[:, hl, :], xdt[:, (h0 + hl) * 64:(h0 + hl + 1) * 64], True, True, ["wTt", "xdt", "cbs"], ["psf5"])
            mm(psf[3][:, :], xc[:, 20 + g, :], S_b[:, g * 512:(g + 1) * 512], True, True, ["xc", "S_b", "dec", "eal", "sm"], ["psf3"])
            V("act", lambda e: e.copy(t1, psf[3][:, :]), ["psf3"], ["t1"])
            V("dve", lambda e, h0=h0: e.tensor_tensor(out=t1.rearrange("p (a b) -> p a b", a=8), in0=t1.rearrange("p (a b) -> p a b", a=8), in1=sm[:, 5, h0:h0 + 8].unsqueeze(2).to_broadcast([128, 8, 64]), op=ALU.mult), ["t1", "sm"], ["t1"])
            V("pool", lambda e, h0=h0, g=g: e.tensor_tensor(out=t2.rearrange("p (a b) -> p a b", a=8), in0=xtok[:, g * 512:(g + 1) * 512].rearrange("p (a b) -> p a b", a=8), in1=D_bc[:, h0:h0 + 8].unsqueeze(2).to_broadcast([128, 8, 64]), op=ALU.mult), ["xtok", "D_bc"], ["t2"])
            V("pool", lambda e: e.tensor_tensor(out=t2, in0=t2, in1=t1, op=ALU.add), ["t1", "t2"], ["t2"])
            V("dve", lambda e: e.tensor_tensor(out=yg, in0=psf[5][:, :], in1=t2, op=ALU.add), ["psf5", "t2"], ["yg"])
            V("dve", lambda e, g=g: e.tensor_tensor(out=yg, in0=yg, in1=zs[:, g * 512:(g + 1) * 512], op=ALU.mult), ["yg", "zs"], ["yg"])
            V("dve", lambda e: e.memset(gsm, 0.0), [], ["gsm"])
            V("act", lambda e: e.activation(out=t1, in_=yg, func=AF.Square, accum_out=gsm[:, 0:1]), ["yg", "gsm", "t1"], ["t1", "gsm"])
            V("dve", lambda e: e.tensor_scalar(out=gsm[:, 1:2], in0=gsm[:, 0:1], scalar1=1.0 / 512, scalar2=EPS, op0=ALU.mult, op1=ALU.add), ["gsm"], ["gsm"])
            V("act", lambda e: e.activation(out=gsm[:, 3:4], in_=gsm[:, 1:2], func=AF.Ln), ["gsm"], ["gsm"])
            V("act", lambda e: e.activation(out=gsm[:, 2:3], in_=gsm[:, 3:4], func=AF.Exp, scale=-0.5), ["gsm"], ["gsm"])
            V("dve", lambda e, g=g: e.scalar_tensor_tensor(out=yn[:, g * 512:(g + 1) * 512], in0=yg, scalar=gsm[:, 2:3], in1=snw_bc[:, g * 512:(g + 1) * 512], op0=ALU.mult, op1=ALU.mult), ["yg", "gsm", "snw_bc"], ["yn"])
            V("pool", lambda e, g=g, h0=h0: e.tensor_tensor(out=xdt[:, g * 512:(g + 1) * 512].rearrange("p (a b) -> p a b", a=8), in0=xdt[:, g * 512:(g + 1) * 512].rearrange("p (a b) -> p a b", a=8), in1=sm[:, 7, h0:h0 + 8].unsqueeze(2).to_broadcast([128, 8, 64]), op=ALU.mult), ["xdt", "sm", "psf5"], ["xdt"])
            mm(psf[4][:, :], Btok[:, g * 128:(g + 1) * 128], xdt[:, g * 512:(g + 1) * 512], True, True, ["Btok", "xdt", "eal", "dec"], ["psf4"])
            V("dve", lambda e, g=g: e.tensor_tensor(out=S_f[:, g * 512:(g + 1) * 512].rearrange("p (a b) -> p a b", a=8), in0=S_f[:, g * 512:(g + 1) * 512].rearrange("p (a b) -> p a b", a=8), in1=eal.unsqueeze(2).to_broadcast([128, 8, 64]), op=ALU.mult), ["S_f", "eal"], ["S_f"])
            V("dve", lambda e, g=g: e.tensor_tensor(out=S_f[:, g * 512:(g + 1) * 512], in0=S_f[:, g * 512:(g + 1) * 512], in1=psf[4][:, :], op=ALU.add), ["S_f", "psf4", "psf3"], ["S_f"])
            V("act", lambda e, g=g: e.copy(S_b[:, g * 512:(g + 1) * 512], S_f[:, g * 512:(g + 1) * 512]), ["S_f", "psf3"], ["S_b"])
            if g == 0:
                def ev_q(pp, pk, j0):
                    V("act", lambda e: e.copy(qraw[:, j0:j0 + 4, :], pp[:, :].rearrange("p (a b) -> p a b", a=4)), [pk], ["qraw"])
                proj_feat(OFF_Q, 1024, ev_q)
                def ev_k(pp, pk, j0):
                    V("act", lambda e: e.mul(kraw[:, j0:j0 + 4, :], pp[:, :].rearrange("p (a b) -> p a b", a=4), 1.0 / 16.0), [pk], ["kraw"])
                proj_feat(OFF_K, 1024, ev_k)
            if g == 1:
                def ev_v(pp, pk, t0, n):
                    V("act", lambda e: e.copy(vtok[:, t0:t0 + n], pp[:, 0:n]), [pk], ["vtok"])
                proj_tok(OFF_V, 2048, ev_v)
            if g == 2:
                def ev_g(pp, pk, t0, n):
                    V("act", lambda e: e.activation(out=gsil[:, t0:t0 + n], in_=pp[:, 0:n], func=AF.Silu), [pk], ["gsil"])
                proj_tok(OFF_G, 2048, ev_g)
            if g == 3:
                def ev_gs(pp, pk, j0):
                    V("act", lambda e: e.activation(out=sgT[:, j0:j0 + 4, :], in_=pp[:, :].rearrange("p (a b) -> p a b", a=4), func=AF.Sigmoid), [pk], ["sgT"])
                proj_feat(OFF_GS, 1024, ev_gs)
                def ev_gr(pp, pk, j0):
                    V("act", lambda e: e.activation(out=srT[:, j0:j0 + 4, :], in_=pp[:, :].rearrange("p (a b) -> p a b", a=4), func=AF.Sigmoid), [pk], ["srT"])
                proj_feat(OFF_GR, 1024, ev_gr)
        for hh in range(2):
            pX = psb[hh].rearrange("p (a b) -> p a b", a=8)
            for j in range(8):
                tr(pX[:, j, :], yn[:, (hh * 8 + j) * 128:(hh * 8 + j + 1) * 128], ident_b, ["yn", "ident_b", "xtok", "xdt", "Btok"], ["psb%d" % hh])
            V("act", lambda e, hh=hh: e.copy(ynT[:, hh * 8:(hh + 1) * 8, :].rearrange("p a b -> p (a b)"), psb[hh][:, :]), ["psb%d" % hh], ["ynT"])
        if dbg and ch == NCH - 1:
            dbg_d["yn"] = dram("dbg_yn", [128, 2048], BF16, kind="ExternalOutput")
            DMA("sp", lambda e: e.dma_start(out=dbg_d["yn"], in_=yn), ["yn"], [])
        barrier()
        A.off = SCR0
        qT = A.alloc([8, 128], BF16)
        kT = A.alloc([8, 128], BF16)
        rt = [A.alloc([4, 128], F32) for _ in range(4)]
        posi = A.alloc([128], I32)
        ang = A.alloc([128], F32)
        rr = A.alloc([128], F32)
        cosT = A.alloc([128], F32)
        sinT = A.alloc([128], F32)
        ktd = A.alloc([1024], BF16)
        sTd = A.alloc([4, 128], BF16)
        qdT = A.alloc([8, 128], BF16)
        yr = A.alloc([2048], BF16)
        ybuf = A.alloc([512], F32)
        ysq = A.alloc([512], F32)
        rs = A.alloc([8], F32)


        DMA("sp", lambda e, tok0=tok0: e.dma_start(out=posi, in_=pos_d[0:1, tok0:tok0 + 128].to_broadcast([128, 128])), [], ["posi"])
        V("dve", lambda e: e.tensor_copy(ang, posi), ["posi"], ["ang"])
        V("dve", lambda e: e.tensor_scalar(out=ang, in0=ang, scalar1=invf[:, 0:1], scalar2=None, op0=ALU.mult), ["ang", "invf"], ["ang"])
        V("dve", lambda e: e.tensor_scalar(out=rr, in0=ang, scalar1=1.0 / (2 * math.pi), scalar2=None, op0=ALU.mult), ["ang"], ["rr"])
        V("dve", lambda e: e.tensor_copy(posi, rr), ["rr", "ang"], ["posi"])
        V("dve", lambda e: e.tensor_copy(rr, posi), ["posi"], ["rr"])
        V("dve", lambda e: e.scalar_tensor_tensor(out=rr, in0=rr, scalar=-2 * math.pi, in1=ang, op0=ALU.mult, op1=ALU.add), ["rr", "ang"], ["rr"])
        V("dve", lambda e: e.tensor_scalar(out=rr, in0=rr, scalar1=3.1415925, scalar2=-3.1415925, op0=ALU.min, op1=ALU.max), ["rr"], ["rr"])
        V("act", lambda e: e.activation(out=sinT, in_=rr, func=AF.Sin), ["rr"], ["sinT"])
        V("dve", lambda e: e.tensor_scalar(out=ang, in0=ang, scalar1=0.5 * math.pi, scalar2=None, op0=ALU.add), ["ang", "sinT", "rr"], ["ang"])
        V("dve", lambda e: e.tensor_scalar(out=rr, in0=ang, scalar1=1.0 / (2 * math.pi), scalar2=None, op0=ALU.mult), ["ang", "sinT"], ["rr"])
        V("dve", lambda e: e.tensor_copy(posi, rr), ["rr"], ["posi"])
        V("dve", lambda e: e.tensor_copy(rr, posi), ["posi"], ["rr"])
        V("dve", lambda e: e.scalar_tensor_tensor(out=rr, in0=rr, scalar=-2 * math.pi, in1=ang, op0=ALU.mult, op1=ALU.add), ["rr", "ang"], ["rr"])
        V("dve", lambda e: e.tensor_scalar(out=rr, in0=rr, scalar1=3.1415925, scalar2=-3.1415925, op0=ALU.min, op1=ALU.max), ["rr"], ["rr"])
        V("act", lambda e: e.activation(out=cosT, in_=rr, func=AF.Sin), ["rr"], ["cosT"])
        cos_b = cosT.unsqueeze(1).to_broadcast([128, 4, 128])
        sin_b = sinT.unsqueeze(1).to_broadcast([128, 4, 128])
        for raw, outT, rk, ok in ((qraw, qT, "qraw", "qT"), (kraw, kT, "kraw", "kT")):
            rv = raw.rearrange("p (h t) n -> p h t n", t=2)
            ov = outT.rearrange("p (h t) n -> p h t n", t=2)
            V("dve", lambda e, rv=rv: e.tensor_tensor(out=rt[0], in0=rv[:, :, 0, :], in1=cos_b, op=ALU.mult), [rk, "cosT", "rt0"], ["rt0"])
            V("pool", lambda e, rv=rv: e.tensor_tensor(out=rt[1], in0=rv[:, :, 1, :], in1=sin_b, op=ALU.mult), [rk, "sinT", "rt1"], ["rt1"])
            V("dve", lambda e, ov=ov: e.tensor_tensor(out=ov[:, :, 0, :], in0=rt[0], in1=rt[1], op=ALU.subtract), ["rt0", "rt1"], [ok])
            V("pool", lambda e, rv=rv: e.tensor_tensor(out=rt[2], in0=rv[:, :, 0, :], in1=sin_b, op=ALU.mult), [rk, "sinT", "rt2"], ["rt2"])
            V("dve", lambda e, rv=rv: e.tensor_tensor(out=rt[3], in0=rv[:, :, 1, :], in1=cos_b, op=ALU.mult), [rk, "cosT", "rt3"], ["rt3"])
            V("dve", lambda e, ov=ov: e.tensor_tensor(out=ov[:, :, 1, :], in0=rt[2], in1=rt[3], op=ALU.add), ["rt2", "rt3"], [ok])
        pX0 = psb[0].rearrange("p (a b) -> p a b", a=8)
        for j in range(8):
            tr(pX0[:, j, :], kT[:, j, :], ident_b, ["kT", "ident_b"], ["psb0"])
        V("act", lambda e: e.copy(ktd, psb[0][:, :]), ["psb0"], ["ktd"])
        V("dve", lambda e: e.tensor_tensor(out=ktd.rearrange("p (a b) -> p a b", a=4), in0=ktd.rearrange("p (a b) -> p a b", a=4), in1=kdec.unsqueeze(2).to_broadcast([128, 4, 256]), op=ALU.mult), ["ktd", "kdec"], ["ktd"])
        qv = qT.rearrange("p (h t) n -> p h t n", t=2)
        qdv = qdT.rearrange("p (h t) n -> p h t n", t=2)
        for t in range(2):
            V("pool", lambda e, t=t: e.tensor_tensor(out=qdv[:, :, t, :], in0=qv[:, :, t, :], in1=qdec_bc, op=ALU.mult), ["qT", "qdec_bc"], ["qdT"])
        for h in range(4):
            for j in range(2):
                mm(psf[3][:, h * 128:(h + 1) * 128], kT[:, 2 * h + j, :], qT[:, 2 * h + j, :], j == 0, j == 1, ["kT", "qT"], ["psf3"])
        V("dve", lambda e: e.tensor_tensor(out=sTd, in0=psf[3][:, :].rearrange("p (a b) -> p a b", a=4), in1=idecT, op=ALU.mult), ["psf3", "idecT"], ["sTd"])
        for h in range(4):
            mm(psf[4][:, :], sTd[:, h, :], vtok[:, h * 512:(h + 1) * 512], True, False, ["sTd", "vtok"], ["psf4"])
            for j in range(2):
                mm(psf[4][:, :], qdT[:, 2 * h + j, :], R_b[:, 2 * h + j, :], False, j == 1, ["qdT", "R_b"], ["psf4"])
            V("dve", lambda e: e.memset(rs, 0.0), [], ["rs"])
            V("act", lambda e: e.activation(out=ybuf, in_=psf[4][:, :], func=AF.Identity, accum_out=rs[:, 0:1]), ["psf4", "rs"], ["ybuf", "rs"])
            V("act", lambda e: e.activation(out=ysq, in_=ybuf, func=AF.Square, accum_out=rs[:, 1:2]), ["ybuf", "rs"], ["ysq", "rs"])
            V("dve", lambda e: e.tensor_scalar(out=rs[:, 2:3], in0=rs[:, 0:1], scalar1=1.0 / 512, scalar2=None, op0=ALU.mult), ["rs"], ["rs"])
            V("dve", lambda e: e.tensor_tensor(out=rs[:, 3:4], in0=rs[:, 2:3], in1=rs[:, 2:3], op=ALU.mult), ["rs"], ["rs"])
            V("dve", lambda e: e.scalar_tensor_tensor(out=rs[:, 4:5], in0=rs[:, 1:2], scalar=1.0 / 512, in1=rs[:, 3:4], op0=ALU.mult, op1=ALU.subtract), ["rs"], ["rs"])
            V("dve", lambda e: e.tensor_scalar(out=rs[:, 4:5], in0=rs[:, 4:5], scalar1=EPS, scalar2=None, op0=ALU.add), ["rs"], ["rs"])
            V("act", lambda e: e.activation(out=rs[:, 5:6], in_=rs[:, 4:5], func=AF.Ln), ["rs"], ["rs"])
            V("act", lambda e: e.activation(out=rs[:, 5:6], in_=rs[:, 5:6], func=AF.Exp, scale=-0.5), ["rs"], ["rs"])
            V("dve", lambda e: e.scalar_tensor_tensor(out=rs[:, 6:7], in0=rs[:, 2:3], scalar=-1.0, in1=rs[:, 5:6], op0=ALU.mult, op1=ALU.mult), ["rs"], ["rs"])
            V("act", lambda e: e.activation(out=ysq, in_=ybuf, func=AF.Identity, bias=rs[:, 6:7], scale=rs[:, 5:6]), ["ybuf", "rs", "ysq"], ["ysq"])
            V("dve", lambda e, h=h: e.tensor_tensor(out=yr[:, h * 512:(h + 1) * 512], in0=ysq, in1=gsil[:, h * 512:(h + 1) * 512], op=ALU.mult), ["ysq", "gsil"], ["yr"])
            for j in range(2):
                mm(psf[5][:, :], ktd[:, h * 256 + j * 128:h * 256 + (j + 1) * 128], vtok[:, h * 512:(h + 1) * 512], True, True, ["ktd", "vtok"], ["psf5"])
                V("dve", lambda e, h=h, j=j: e.scalar_tensor_tensor(out=R_f[:, 2 * h + j, :], in0=R_f[:, 2 * h + j, :], scalar=cdec[h], in1=psf[5][:, :], op0=ALU.mult, op1=ALU.add), ["R_f", "psf5"], ["R_f"])
                V("act", lambda e, h=h, j=j: e.copy(R_b[:, 2 * h + j, :], R_f[:, 2 * h + j, :]), ["R_f", "psf4"], ["R_b"])
        for hh in range(2):
            pX = psb[hh].rearrange("p (a b) -> p a b", a=8)
            for j in range(8):
                tr(pX[:, j, :], yr[:, (hh * 8 + j) * 128:(hh * 8 + j + 1) * 128], ident_b, ["yr", "ident_b", "ktd"], ["psb%d" % hh])
            V("act", lambda e, hh=hh: e.copy(yrT[:, hh * 8:(hh + 1) * 8, :].rearrange("p a b -> p (a b)"), psb[hh][:, :]), ["psb%d" % hh], ["yrT"])
        if dbg and ch == NCH - 1:
            dbg_d["yr"] = dram("dbg_yr", [128, 2048], BF16, kind="ExternalOutput")
            DMA("sp", lambda e: e.dma_start(out=dbg_d["yr"], in_=yr), ["yr"], [])
        barrier()
        A.off = SCR0
        mA = A.alloc([8, 128], F32)
        mB = A.alloc([8, 128], F32)
        mT = A.alloc([8, 128], BF16)
        xt2 = A.alloc([D], F32)
        x2 = A.alloc([D], F32)
        xn2 = A.alloc([D], F32)
        h2f = A.alloc([8, 128], F32)
        h2b = A.alloc([8, 128], BF16)
        ss2 = A.alloc([4], F32)
        scr_ = A.alloc([NE], F32)
        sel = A.alloc([NE], F32)
        selm = A.alloc([NE], F32)
        wsel = A.alloc([NE], F32)
        m8all = A.alloc([8, 8], F32)
        gsc = A.alloc([8], F32)
        m8 = A.alloc([8], F32)
        gmask = A.alloc([8], F32)
        gm10 = A.alloc([8], F32)
        den = A.alloc([2], F32)


        for which, (wsrc, wkeys, inT, ink, gT, gk) in enumerate(((wsob_d, WSOB_KEYS, ynT, "ynT", sgT, "sgT"), (wrob_d, WROB_KEYS, yrT, "yrT", srT, "srT"))):
            for nh in range(2):
                pp, pk = next_pj()
                bb = [load_w(wsrc, wkeys, kg * 1024, nh * 512, 512) for kg in range(2)]
                for j in range(4):
                    for kg in range(2):
                        b, bk = bb[kg]
                        for k in range(8):
                            mm(pp[:, j * 128:(j + 1) * 128], b[:, k, j * 128:(j + 1) * 128], inT[:, kg * 8 + k, :], kg == 0 and k == 0, kg == 1 and k == 7, [ink, bk], [pk])
                dst = mA if which == 0 else mB
                V("dve", lambda e, dst=dst, pp=pp, gT=gT, nh=nh: e.tensor_tensor(out=dst[:, nh * 4:(nh + 1) * 4, :], in0=pp[:, :].rearrange("p (a b) -> p a b", a=4), in1=gT[:, nh * 4:(nh + 1) * 4, :], op=ALU.mult), [pk, gk], ["mA" if which == 0 else "mB"])
        V("dve", lambda e: e.tensor_tensor(out=mT, in0=mA, in1=mB, op=ALU.add), ["mA", "mB"], ["mT"])
        DMA("sp", lambda e, tok0=tok0: e.dma_start(out=xt2, in_=x_d[tok0:tok0 + 128, :]), [], ["xt2"])
        for nh in range(2):
            b, bk = load_w(wob_d, WOB_KEYS, 0, nh * 512, 512)
            pp, pk = next_pj()
            for k in range(8):
                mm(pp[:, :], mT[:, k, :], b[:, k, :], k == 0, k == 7, ["mT", bk], [pk])
            V("dve", lambda e, pp=pp, nh=nh: e.tensor_tensor(out=x2[:, nh * 512:(nh + 1) * 512], in0=pp[:, :], in1=g1_bc[:, nh * 512:(nh + 1) * 512], op=ALU.mult), [pk, "g1_bc"], ["x2"])
        V("dve", lambda e: e.tensor_tensor(out=x2, in0=x2, in1=xt2, op=ALU.add), ["x2", "xt2"], ["x2"])
        DMA("sp", lambda e, tok0=tok0: e.dma_start(out=x2_d[tok0:tok0 + 128, :], in_=x2), ["x2"], [("x2d", ch)])
        if dbg and ch == NCH - 1:
            dbg_d["x2"] = dram("dbg_x2", [128, D], F32, kind="ExternalOutput")
            DMA("sp", lambda e: e.dma_start(out=dbg_d["x2"], in_=x2), ["x2"], [])
            dbg_d["mT"] = dram("dbg_mT", [128, 8, 128], BF16, kind="ExternalOutput")
            DMA("sp", lambda e: e.dma_start(out=dbg_d["mT"], in_=mT), ["mT"], [])
        V("dve", lambda e: e.memset(ss2, 0.0), [], ["ss2"])
        V("act", lambda e: e.activation(out=xn2, in_=x2, func=AF.Square, accum_out=ss2[:, 0:1]), ["x2", "ss2"], ["xn2", "ss2"])
        V("dve", lambda e: e.tensor_scalar(out=ss2[:, 1:2], in0=ss2[:, 0:1], scalar1=1.0 / D, scalar2=EPS, op0=ALU.mult, op1=ALU.add), ["ss2"], ["ss2"])
        V("act", lambda e: e.activation(out=ss2[:, 3:4], in_=ss2[:, 1:2], func=AF.Ln), ["ss2"], ["ss2"])
        V("act", lambda e: e.activation(out=ss2[:, 2:3], in_=ss2[:, 3:4], func=AF.Exp, scale=-0.5), ["ss2"], ["ss2"])
        V("dve", lambda e: e.tensor_scalar(out=xn2, in0=x2, scalar1=ss2[:, 2:3], scalar2=None, op0=ALU.mult), ["x2", "ss2", "xn2"], ["xn2"])
        for k in range(8):
            tr(psf[3 + k // 4][:, (k % 4) * 128:(k % 4 + 1) * 128], xn2[:, k * 128:(k + 1) * 128], ident_f, ["xn2", "ident_f"], ["psf%d" % (3 + k // 4)])
        for k in range(8):
            V("act", lambda e, k=k: e.activation(out=h2f[:, k, :], in_=psf[3 + k // 4][:, (k % 4) * 128:(k % 4 + 1) * 128], func=AF.Identity, bias=sh2col[:, k:k + 1], scale=s2col[:, k:k + 1]), ["psf%d" % (3 + k // 4), "s2col", "sh2col"], ["h2f"])
        V("dve", lambda e: e.tensor_copy(h2b, h2f), ["h2f"], ["h2b"])
        DMA("sp", lambda e, tok0=tok0: e.dma_start(out=h2T_d[:, :, tok0:tok0 + 128], in_=h2b), ["h2b"], [("h2Td", ch)])
        wr_sb = A.alloc([8, NE], F32)
        DMA("sp", lambda e, wr_sb=wr_sb: e.dma_start(out=wr_sb, in_=wr_d.rearrange("(k p) n -> p k n", p=128)), [], ["wr_sb"])
        for k in range(8):
            mm(psf[5][:, 0:NE], h2f[:, k, :], wr_sb[:, k, :], k == 0, k == 7, ["h2f", "wr_sb"], ["psf5"])
        V("act", lambda e: e.activation(out=scr_, in_=psf[5][:, 0:NE], func=AF.Sigmoid), ["psf5"], ["scr_"])
        V("dve", lambda e: e.tensor_tensor(out=sel, in0=scr_, in1=rb_bc, op=ALU.add), ["scr_", "rb_bc"], ["sel"])
        for g in range(8):
            V("dve", lambda e, g=g: e.max(out=m8all[:, g, :], in_=sel[:, g * 32:(g + 1) * 32]), ["sel"], ["m8all"])
        V("dve", lambda e: e.tensor_tensor(out=gsc, in0=m8all[:, :, 0], in1=m8all[:, :, 1], op=ALU.add), ["m8all"], ["gsc"])
        V("dve", lambda e: e.max(out=m8, in_=gsc), ["gsc"], ["m8"])
        V("dve", lambda e: e.tensor_scalar(out=gmask, in0=gsc, scalar1=m8[:, 3:4], scalar2=None, op0=ALU.is_ge), ["gsc", "m8"], ["gmask"])
        V("dve", lambda e: e.tensor_scalar(out=gm10, in0=gmask, scalar1=10.0, scalar2=-10.0, op0=ALU.mult, op1=ALU.add), ["gmask"], ["gm10"])
        V("dve", lambda e: e.tensor_tensor(out=selm.rearrange("p (a b) -> p a b", a=8), in0=sel.rearrange("p (a b) -> p a b", a=8), in1=gmask.unsqueeze(2).to_broadcast([128, 8, 32]), op=ALU.mult), ["sel", "gmask"], ["selm"])
        V("dve", lambda e: e.tensor_tensor(out=selm.rearrange("p (a b) -> p a b", a=8), in0=selm.rearrange("p (a b) -> p a b", a=8), in1=gm10.unsqueeze(2).to_broadcast([128, 8, 32]), op=ALU.add), ["selm", "gm10"], ["selm"])
        V("dve", lambda e: e.max(out=m8, in_=selm), ["selm", "gmask"], ["m8"])
        V("dve", lambda e: e.memset(den, 0.0), [], ["den"])
        V("dve", lambda e: e.scalar_tensor_tensor(out=wsel, in0=selm, scalar=m8[:, 7:8], in1=scr_, op0=ALU.is_ge, op1=ALU.mult, accum_out=den[:, 0:1]), ["selm", "m8", "scr_", "den"], ["wsel", "den"])
        V("dve", lambda e: e.tensor_scalar(out=den[:, 1:2], in0=den[:, 0:1], scalar1=1e-20, scalar2=None, op0=ALU.add), ["den"], ["den"])
        V("dve", lambda e: e.reciprocal(den[:, 1:2], den[:, 1:2]), ["den"], ["den"])
        V("dve", lambda e: e.tensor_scalar(out=wsel, in0=wsel, scalar1=den[:, 1:2], scalar2=2.5, op0=ALU.mult, op1=ALU.mult), ["wsel", "den"], ["wsel"])
        Mf = A.alloc([NE], F32)
        Mb = A.alloc([NE], BF16)
        posf = A.alloc([NE], F32)
        keyf = A.alloc([NE], F32)
        kjunk = A.alloc([NE], F32)
        k8 = A.alloc([8], F32)
        k8i = A.alloc([8], I32)
        t8i = A.alloc([8], I32)
        t8j = A.alloc([8], I32)
        h2tok = A.alloc([D], BF16)
        V("dve", lambda e: e.tensor_single_scalar(Mf, wsel, 0.0, op=ALU.is_gt), ["wsel"], ["Mf"])
        V("dve", lambda e: e.tensor_copy(Mb, Mf), ["Mf"], ["Mb"])
        mm(psf[3][:, 0:NE], striu_b, Mb, True, True, ["striu_b", "Mb"], ["psf3"])
        mm(psf[4][:, 0:NE], ones_b, Mb, True, True, ["ones_b", "Mb"], ["psf4"])
        V("dve", lambda e: e.tensor_tensor(out=posf, in0=psf[3][:, 0:NE], in1=run_bc, op=ALU.add), ["psf3", "run_bc"], ["posf"])
        V("dve", lambda e: e.tensor_tensor(out=run_bc, in0=psf[4][:, 0:NE], in1=run_bc, op=ALU.add), ["psf4", "run_bc"], ["run_bc"])
        V("dve", lambda e: e.tensor_tensor(out=keyf, in0=posf, in1=iota8k, op=ALU.add), ["posf", "iota8k"], ["keyf"])
        V("dve", lambda e: e.scalar_tensor_tensor(out=keyf, in0=keyf, scalar=1.0, in1=Mf, op0=ALU.add, op1=ALU.mult), ["keyf", "Mf"], ["keyf"])
        V("dve", lambda e: e.tensor_scalar(out=keyf, in0=keyf, scalar1=-1.0, scalar2=None, op0=ALU.add), ["keyf"], ["keyf"])
        V("dve", lambda e: e.max(out=k8, in_=keyf), ["keyf"], ["k8"])
        V("dve", lambda e: e.tensor_copy(k8i, k8), ["k8"], ["k8i"])
        V("dve", lambda e: e.tensor_scalar(out=t8i, in0=k8i, scalar1=13, scalar2=None, op0=ALU.arith_shift_right), ["k8i"], ["t8i"])
        V("dve", lambda e, ch=ch: e.tensor_copy(e8s[:, ch, :], t8i), ["t8i"], ["e8s"])
        V("dve", lambda e: e.tensor_scalar(out=t8j, in0=k8i, scalar1=8191, scalar2=None, op0=ALU.bitwise_and), ["k8i"], ["t8j"])
        V("dve", lambda e, ch=ch: e.tensor_copy(pos8s[:, ch, :], t8j), ["t8j"], ["pos8s"])
        for k in range(8):
            V("dve", lambda e, k=k, ch=ch: e.scalar_tensor_tensor(out=kjunk, in0=keyf, scalar=k8[:, k:k + 1], in1=wsel, op0=ALU.is_equal, op1=ALU.mult, accum_out=w8s[:, ch, k:k + 1]), ["keyf", "k8", "wsel", "w8s", "kjunk"], ["kjunk", "w8s"])
        pXh = psb[0].rearrange("p (a b) -> p a b", a=8)
        for k in range(8):
            tr(pXh[:, k, :], h2b[:, k, :], ident_b, ["h2b", "ident_b"], ["psb0"])
        V("act", lambda e: e.copy(h2tok, psb[0][:, :]), ["psb0"], ["h2tok"])
        DMA("sp", lambda e, tok0=tok0: e.dma_start(out=h2tok_d[tok0:tok0 + 128, :], in_=h2tok), ["h2tok"], [("h2tokd", ch)])
        barrier()
        A.off = SCR0

    A.off = MARK2
    IO = lambda ap: bass.IndirectOffsetOnAxis(ap=ap, axis=0)
    widx_i = A.alloc([NB], I32)
    gidx = A.alloc([NB], I32)
    ridx = A.alloc([NB], I32)
    wcol_raw = A.alloc([NB], I32)
    wcol = wcol_raw.bitcast(F32)
    MARK3 = A.off
    padf = A.alloc([NE], F32)
    padi = A.alloc([NE], I32)
    pend = A.alloc([NE], F32)
    pstart = A.alloc([NE], F32)
    onesrow = A.alloc([NE], F32)
    Dk = [A.alloc([128], F32) for _ in range(2)]
    pcol2 = A.alloc([2], F32)
    bvals_i = A.alloc([NB], I32)
    bvals = A.alloc([NB], F32)
    ind = [A.alloc([NB], BF16) for _ in range(2)]
    widx_f = A.alloc([NB], F32)
    tabtmp = A.alloc([NB], I32)
    tab0 = A.alloc([NB, 2], I32)
    zrow = A.alloc([D], BF16)
    pst8s = A.alloc([NCH, 8], F32)
    slot_f = A.alloc([NCH, 8], F32)
    slot_i = A.alloc([NCH, 8], I32)
    sp_i = A.alloc([NCH, 8], I32)
    sb_i = A.alloc([NCH, 8], I32)
    sp_f = A.alloc([NCH, 8], F32)
    sb_f = A.alloc([NCH, 8], F32)
    td_i = A.alloc([NCH, 8], I32)
    rowb_i = A.alloc([8], I32)
    rec_raw = A.alloc([NCH * 16], F32)
    recf = rec_raw.rearrange("p (c k w) -> p c k w", c=NCH, k=8)
    reci = rec_raw.bitcast(I32).rearrange("p (c k w) -> p c k w", c=NCH, k=8)
    kj2 = A.alloc([NE], F32)
    tab_sb = A.alloc([NB, 2], I32)
    V("dve", lambda e: e.tensor_scalar(out=padf, in0=run_bc, scalar1=127.0, scalar2=None, op0=ALU.add), ["run_bc"], ["padf"])
    V("dve", lambda e: e.tensor_copy(padi, padf), ["padf"], ["padi"])
    V("dve", lambda e: e.tensor_scalar(out=padi, in0=padi, scalar1=7, scalar2=7, op0=ALU.arith_shift_right, op1=ALU.logical_shift_left), ["padi"], ["padi"])
    V("dve", lambda e: e.tensor_copy(padf, padi), ["padi"], ["padf"])
    V("dve", lambda e: e.memset(onesrow, 1.0), [], ["onesrow"])
    V("dve", lambda e: e.tensor_tensor_scan(out=pend, data0=onesrow, data1=padf, initial=0.0, op0=ALU.mult, op1=ALU.add), ["onesrow", "padf"], ["pend"])
    V("dve", lambda e: e.tensor_tensor(out=pstart, in0=pend, in1=padf, op=ALU.subtract), ["pend", "padf"], ["pstart"])
    for k in range(2):
        V("dve", lambda e, k=k: e.tensor_tensor(out=Dk[k], in0=ident_f, in1=pend[:, k * 128:(k + 1) * 128], op=ALU.mult), ["ident_f", "pend"], ["Dk%d" % k])
        mm(psf[0][:, k:k + 1], Dk[k], ones_f[:, 0:1], True, True, ["Dk%d" % k, "ones_f"], ["psf0"])
    V("dve", lambda e: e.tensor_copy(pcol2, psf[0][:, 0:2]), ["psf0"], ["pcol2"])
    V("pool", lambda e: e.iota(bvals_i, pattern=[[128, NB]], base=0, channel_multiplier=0), [], ["bvals_i"])
    V("dve", lambda e: e.tensor_copy(bvals, bvals_i), ["bvals_i"], ["bvals"])
    for k in range(2):
        V("dve", lambda e, k=k: e.tensor_scalar(out=ind[k], in0=bvals, scalar1=pcol2[:, k:k + 1], scalar2=None, op0=ALU.is_ge), ["bvals", "pcol2"], ["ind%d" % k])
        mm(psf[1][:, 0:NB], ones_b, ind[k], k == 0, k == 1, ["ones_b", "ind%d" % k], ["psf1"])
    V("dve", lambda e: e.tensor_scalar(out=widx_f, in0=psf[1][:, 0:NB], scalar1=128.0, scalar2=pcol[:, 0:1], op0=ALU.mult, op1=ALU.add), ["psf1", "pcol"], ["widx_f"])
    V("dve", lambda e: e.tensor_copy(widx_i, widx_f), ["widx_f"], ["widx_i"])
    V("pool", lambda e: e.iota(tabtmp, pattern=[[0, NB]], base=S * 8 + 128, channel_multiplier=0), [], ["tabtmp"])
    V("dve", lambda e: e.memset(tab0, 0), [], ["tab0"])
    V("dve", lambda e: e.tensor_copy(tab0[:, :, 0], tabtmp), ["tabtmp", "tab0"], ["tab0"])
    DMA("sp", lambda e: e.dma_start(out=tab_d.rearrange("(p b) w -> p (b w)", p=128), in_=tab0.rearrange("p b w -> p (b w)")), ["tab0"], ["tabinit"])
    V("dve", lambda e: e.memset(zrow, 0.0), [], ["zrow"])
    DMA("sp", lambda e: e.dma_start(out=h2tok_d[S:S + 16, :], in_=zrow[0:16, :]), ["zrow"], [("h2tokd", "z")])
    V("pool", lambda e: e.iota(rowb_i, pattern=[[1, 8]], base=0, channel_multiplier=8), [], ["rowb_i"])
    V("dve", lambda e: e.memset(pst8s, 0.0), [], ["pst8s"])
    for c in range(NCH):
        for k in range(8):
            V("dve", lambda e, c=c, k=k: e.scalar_tensor_tensor(out=kj2, in0=iota_e, scalar=e8s[:, c, k:k + 1], in1=pstart, op0=ALU.is_equal, op1=ALU.mult, accum_out=pst8s[:, c, k:k + 1]), ["iota_e", "e8s", "pstart", "pst8s", "kj2"], ["kj2", "pst8s"])
    V("dve", lambda e: e.tensor_tensor(out=slot_f, in0=pst8s, in1=pos8s, op=ALU.add), ["pst8s", "pos8s"], ["slot_f"])
    V("dve", lambda e: e.tensor_copy(slot_i, slot_f), ["slot_f"], ["slot_i"])
    V("dve", lambda e: e.tensor_scalar(out=sp_i, in0=slot_i, scalar1=127, scalar2=None, op0=ALU.bitwise_and), ["slot_i"], ["sp_i"])
    V("dve", lambda e: e.tensor_scalar(out=sb_i, in0=slot_i, scalar1=7, scalar2=None, op0=ALU.arith_shift_right), ["slot_i"], ["sb_i"])
    V("dve", lambda e: e.tensor_copy(sp_f, sp_i), ["sp_i"], ["sp_f"])
    V("dve", lambda e: e.tensor_copy(sb_f, sb_i), ["sb_i"], ["sb_f"])
    V("dve", lambda e: e.scalar_tensor_tensor(out=slot_f, in0=sp_f, scalar=float(NB), in1=sb_f, op0=ALU.mult, op1=ALU.add), ["sp_f", "sb_f", "slot_i"], ["slot_f"])
    V("dve", lambda e: e.tensor_copy(td_i, slot_f), ["slot_f"], ["td_i"])
    for c in range(NCH):
        V("dve", lambda e, c=c: e.tensor_scalar(out=reci[:, c, :, 0], in0=rowb_i, scalar1=c * 128 * 8, scalar2=None, op0=ALU.add), ["rowb_i"], ["rec"])
    V("dve", lambda e: e.tensor_copy(recf[:, :, :, 1], w8s), ["w8s", "rec"], ["rec"])
    SC_KEYS = []
    for c in range(NCH):
        for k in range(8):
            DMA("pool", lambda e, c=c, k=k: e.indirect_dma_start(out=tab_d, out_offset=IO(td_i[:, c, k:k + 1]), in_=reci[:, c, k, :], in_offset=None), ["rec", "td_i", "tabinit"], [("tabsc", c, k)])
            SC_KEYS.append(("tabsc", c, k))
    DMA("sp", lambda e: e.dma_start(out=tab_sb.rearrange("p b w -> p (b w)"), in_=tab_d.rearrange("(p b) w -> p (b w)", p=128)), SC_KEYS + ["tabinit"], ["tab_sb"])
    V("dve", lambda e: e.tensor_copy(ridx, tab_sb[:, :, 0]), ["tab_sb"], ["ridx"])
    V("dve", lambda e: e.tensor_scalar(out=gidx, in0=ridx, scalar1=3, scalar2=None, op0=ALU.arith_shift_right), ["ridx"], ["gidx"])
    V("dve", lambda e: e.tensor_copy(wcol_raw, tab_sb[:, :, 1]), ["tab_sb"], ["wcol"])
    barrier()
    A.off = MARK3
    NBUF = 3
    wall = [A.alloc([6144], BF16) for _ in range(NBUF)]
    wgb = [w[:, 0:2048] for w in wall]
    wub = [w[:, 2048:4096] for w in wall]
    wdb = [w[:, 4096:6144] for w in wall]
    Xe = [A.alloc([D], BF16) for _ in range(NBUF)]
    XeT = [A.alloc([8, 128], BF16) for _ in range(2)]
    hgb = [A.alloc([256], F32) for _ in range(2)]
    hidTb = [A.alloc([2, 128], BF16) for _ in range(2)]
    Ye = [A.alloc([D], F32) for _ in range(2)]
    H2TOK_KEYS = [("h2tokd", c) for c in range(NCH)] + [("h2tokd", "z")]
    ALLH = [("h2Td", c) for c in range(NCH)]
    wsg_sb = A.alloc([8, 256], BF16)
    wsu_sb = A.alloc([8, 256], BF16)
    wsd_sb = A.alloc([2, D], BF16)
    h2t = [A.alloc([8, 128], BF16) for _ in range(2)]
    shd_sb = [A.alloc([D], F32) for _ in range(2)]
    DMA("pool", lambda e: e.dma_start(out=wsg_sb, in_=wsg_d.rearrange("(k p) n -> p k n", p=128)), [], ["wsg_sb"])
    DMA("pool", lambda e: e.dma_start(out=wsu_sb, in_=wsu_d.rearrange("(k p) n -> p k n", p=128)), [], ["wsu_sb"])
    DMA("pool", lambda e: e.dma_start(out=wsd_sb, in_=wsd_d.rearrange("(k p) n -> p k n", p=128)), [], ["wsd_sb"])
    SH_EVERY = max(1, NB // NCH)
    SHKEYS = []

    def shared_tile(ti):
        j = ti % 2
        t0 = ti * 128
        DMA("sp", lambda e: e.dma_start(out=h2t[j], in_=h2T_d[:, :, t0:t0 + 128]), ALLH, ["h2t%d" % j])
        pgu = psf[j]
        for m in range(4):
            wsrc_ = wsg_sb if m < 2 else wsu_sb
            wk_ = "wsg_sb" if m < 2 else "wsu_sb"
            for k in range(8):
                mm(pgu[:, m * 128:(m + 1) * 128], wsrc_[:, k, (m % 2) * 128:(m % 2 + 1) * 128], h2t[j][:, k, :], k == 0, k == 7, [wk_, "h2t%d" % j], ["psf%d" % j])
        V("act", lambda e: e.activation(out=hgb[j], in_=pgu[:, 0:256], func=AF.Silu), ["psf%d" % j], ["hg%d" % j])
        V("dve", lambda e: e.tensor_tensor(out=hidTb[j].rearrange("p a b -> p (a b)"), in0=pgu[:, 256:512], in1=hgb[j], op=ALU.mult), ["psf%d" % j, "hg%d" % j], ["hidT%d" % j])
        for nh in range(2):
            pd = psf[2 + 2 * j + nh]
            pdk = "psf%d" % (2 + 2 * j + nh)
            for kc in range(2):
                mm(pd[:, :], hidTb[j][:, kc, :], wsd_sb[:, kc, nh * 512:(nh + 1) * 512], kc == 0, kc == 1, ["hidT%d" % j, "wsd_sb"], [pdk])
            V("act", lambda e, nh=nh, pd=pd: e.copy(shd_sb[j][:, nh * 512:(nh + 1) * 512], pd[:, :]), [pdk], ["shd_sb%d" % j])
        DMA("sp", lambda e: e.dma_start(out=shd_d[t0:t0 + 128, :], in_=shd_sb[j]), ["shd_sb%d" % j], [("shd", ti)])
        SHKEYS.append(("shd", ti))
    WMAX = NE * 128 - 1
    bc_reg = {}

    def gathers(b):
        i = b % NBUF
        def wgather(e):
            if "r" not in bc_reg:
                bc_reg["r"] = e.to_reg(WMAX)
            return e.indirect_dma_start(out=wall[i], out_offset=None, in_=weL_d, in_offset=IO(widx_i[:, b:b + 1]), bounds_check=bc_reg["r"], oob_is_err=False)
        DMA("pool", wgather, ["widx_i"], ["wall%d" % i])
        def xgather(e):
            if "x" not in bc_reg:
                bc_reg["x"] = e.to_reg(S + 15)
            return e.indirect_dma_start(out=Xe[i], out_offset=None, in_=h2tok_d, in_offset=IO(gidx[:, b:b + 1]), bounds_check=bc_reg["x"], oob_is_err=False)
        DMA("pool", xgather, ["gidx"] + H2TOK_KEYS, ["Xe%d" % i])

    YKEYS = []
    for i_ in range(NBUF):
        V("dve", lambda e, i_=i_: e.memset(Xe[i_], 0.0), [], ["Xe%d" % i_])
    for b in range(min(NBUF, NB)):
        gathers(b)
    for b in range(NB):
        i = b % NBUF
        j = b % 2
        pXe = psb[j].rearrange("p (a b) -> p a b", a=8)
        for k in range(8):
            tr(pXe[:, k, :], Xe[i][:, k * 128:(k + 1) * 128], ident_b, ["Xe%d" % i, "ident_b"], ["psb%d" % j])
        V("dve" if j == 0 else "act", (lambda j: (lambda e: e.tensor_copy(XeT[j].rearrange("p a b -> p (a b)"), psb[j][:, :])) if j == 0 else (lambda e: e.copy(XeT[j].rearrange("p a b -> p (a b)"), psb[j][:, :])))(j), ["psb%d" % j], ["XeT%d" % j])
        pgu = psf[j]
        for m in range(4):
            wsrc_ = wgb[i] if m < 2 else wub[i]
            wk_ = "wall%d" % i
            for k in range(8):
                mm(pgu[:, m * 128:(m + 1) * 128], wsrc_[:, k * 256 + (m % 2) * 128:k * 256 + (m % 2 + 1) * 128], XeT[j][:, k, :], k == 0, k == 7, [wk_, "XeT%d" % j], ["psf%d" % j])
        V("act", lambda e, j=j, pgu=pgu: e.activation(out=hgb[j], in_=pgu[:, 0:256], func=AF.Silu), ["psf%d" % j], ["hg%d" % j])
        V("dve", lambda e, j=j, pgu=pgu: e.tensor_tensor(out=hidTb[j].rearrange("p a b -> p (a b)"), in0=pgu[:, 256:512], in1=hgb[j], op=ALU.mult), ["psf%d" % j, "hg%d" % j], ["hidT%d" % j])
        for nh in range(2):
            pd = psf[2 + 2 * j + nh]
            pdk = "psf%d" % (2 + 2 * j + nh)
            for kc in range(2):
                mm(pd[:, :], hidTb[j][:, kc, :], wdb[i][:, kc * 1024 + nh * 512:kc * 1024 + (nh + 1) * 512], kc == 0, kc == 1, ["hidT%d" % j, "wall%d" % i], [pdk])
            V("act", lambda e, j=j, nh=nh, pd=pd, b=b: e.activation(out=Ye[j][:, nh * 512:(nh + 1) * 512], in_=pd[:, :], func=AF.Copy, scale=wcol[:, b:b + 1]), [pdk, "wcol"], ["Ye%d" % j])
        if b + NBUF < NB:
            gathers(b + NBUF)
        def yscatter(e, j=j, b=b):
            if "y" not in bc_reg:
                bc_reg["y"] = e.to_reg(S * 8 + 127)
            return e.indirect_dma_start(out=yall_d, out_offset=IO(ridx[:, b:b + 1]), in_=Ye[j], in_offset=None, bounds_check=bc_reg["y"], oob_is_err=False)
        DMA("pool", yscatter, ["Ye%d" % j, "ridx"], [("yall", b)])
        YKEYS.append(("yall", b))
        if (b + 1) % SH_EVERY == 0 and (b + 1) // SH_EVERY <= NCH:
            shared_tile((b + 1) // SH_EVERY - 1)
    barrier()
    assert len(SHKEYS) == NCH
    A.off = MARK2
    Yt2 = [A.alloc([8, D], F32) for _ in range(2)]
    x2t2 = [A.alloc([D], F32) for _ in range(2)]
    sht2 = [A.alloc([D], F32) for _ in range(2)]
    x3 = A.alloc([D], F32)
    ojunk2 = [A.alloc([D], F32) for _ in range(2)]
    fs = A.alloc([4], F32)
    fnw_bc = A.alloc([D], F32)
    DMA("sp", lambda e: e.dma_start(out=fnw_bc, in_=fnw_d.to_broadcast([128, D])), [], ["fnw_bc"])
    ALLX = [("x2d", c) for c in range(NCH)]
    for ti in range(NCH):
        t0 = ti * 128
        j = ti % 2
        Yt, x2t, sht, ojunk = Yt2[j], x2t2[j], sht2[j], ojunk2[j]
        DMA("sp", lambda e, t0=t0, Yt=Yt: e.dma_start(out=Yt, in_=yall_d[t0 * 8:(t0 + 128) * 8, :].rearrange("(p k) d -> p k d", k=8)), YKEYS, ["Yt%d" % j])
        DMA("sp", lambda e, t0=t0, x2t=x2t: e.dma_start(out=x2t, in_=x2_d[t0:t0 + 128, :]), ALLX, ["x2t%d" % j])
        DMA("sp", lambda e, t0=t0, sht=sht: e.dma_start(out=sht, in_=shd_d[t0:t0 + 128, :]), SHKEYS, ["sht%d" % j])
        V("pool", lambda e, Yt=Yt: e.tensor_tensor(out=Yt[:, 0:4, :], in0=Yt[:, 0:4, :], in1=Yt[:, 4:8, :], op=ALU.add), ["Yt%d" % j], ["Yt%d" % j])
        V("dve", lambda e, Yt=Yt: e.tensor_tensor(out=Yt[:, 0:2, :], in0=Yt[:, 0:2, :], in1=Yt[:, 2:4, :], op=ALU.add), ["Yt%d" % j], ["Yt%d" % j])
        V("dve", lambda e, Yt=Yt: e.tensor_tensor(out=Yt[:, 0, :], in0=Yt[:, 0, :], in1=Yt[:, 1, :], op=ALU.add), ["Yt%d" % j], ["Yt%d" % j])
        V("dve", lambda e, Yt=Yt, sht=sht: e.tensor_tensor(out=x3, in0=sht, in1=Yt[:, 0, :], op=ALU.add), ["sht%d" % j, "Yt%d" % j], ["x3"])
        V("dve", lambda e: e.tensor_tensor(out=x3, in0=x3, in1=g2_bc, op=ALU.mult), ["x3", "g2_bc"], ["x3"])
        V("dve", lambda e, x2t=x2t: e.tensor_tensor(out=x3, in0=x3, in1=x2t, op=ALU.add), ["x3", "x2t%d" % j], ["x3"])
        V("dve", lambda e: e.memset(fs, 0.0), [], ["fs"])
        V("act", lambda e, ojunk=ojunk: e.activation(out=ojunk, in_=x3, func=AF.Square, accum_out=fs[:, 0:1]), ["x3", "fs"], ["ojunk%d" % j, "fs"])
        V("dve", lambda e: e.tensor_scalar(out=fs[:, 1:2], in0=fs[:, 0:1], scalar1=1.0 / D, scalar2=EPS, op0=ALU.mult, op1=ALU.add), ["fs"], ["fs"])
        V("act", lambda e: e.activation(out=fs[:, 3:4], in_=fs[:, 1:2], func=AF.Ln), ["fs"], ["fs"])
        V("act", lambda e: e.activation(out=fs[:, 2:3], in_=fs[:, 3:4], func=AF.Exp, scale=-0.5), ["fs"], ["fs"])
        V("dve", lambda e, ojunk=ojunk: e.scalar_tensor_tensor(out=ojunk, in0=x3, scalar=fs[:, 2:3], in1=fnw_bc, op0=ALU.mult, op1=ALU.mult), ["x3", "fs", "fnw_bc", "ojunk%d" % j], ["ojunk%d" % j])
        DMA("sp", lambda e, t0=t0, ojunk=ojunk: e.dma_start(out=out_d[t0:t0 + 128, :], in_=ojunk), ["ojunk%d" % j], [("outd", t0)])
    return locals()


def _host_inputs(inp, b, S):
    f = lambda a: np.ascontiguousarray(np.asarray(a, dtype=np.float32))
    col = lambda v: f(np.asarray(v).reshape(8, 128).T)
    weL = np.ascontiguousarray(np.concatenate([
        np.asarray(inp["w_exp_gate"][0], dtype=np.float32).reshape(256, 8, 128, 256).transpose(0, 2, 1, 3).reshape(256 * 128, 2048),
        np.asarray(inp["w_exp_up"][0], dtype=np.float32).reshape(256, 8, 128, 256).transpose(0, 2, 1, 3).reshape(256 * 128, 2048),
        np.asarray(inp["w_exp_down"][0], dtype=np.float32).reshape(256, 2, 128, 1024).transpose(0, 2, 1, 3).reshape(256 * 128, 2048)], axis=1))
    return {
        "x": f(inp["x"][b, :S]), "pos": np.ascontiguousarray(np.asarray(inp["positions"][b, :S], dtype=np.int32).reshape(1, S)),
        "ccol": col(inp["c"][b]), "w_ada": f(inp["w_ada"][0]), "b_ada": f(np.asarray(inp["b_ada"][0]).reshape(1, -1)),
        "n1col": col(inp["norm1_w"][0]), "n2col": col(inp["norm2_w"][0]), "w_in": f(inp["w_in"][0]),
        "convw": f(np.asarray(inp["conv_w"][0]).T.reshape(24, 128, 4).transpose(1, 0, 2)),
        "convb": f(np.asarray(inp["conv_b"][0]).reshape(24, 128).T),
        "dt_bias": f(np.asarray(inp["dt_bias"][0]).reshape(1, 32)), "a_log": f(np.asarray(inp["a_log"][0]).reshape(1, 32)),
        "d_skip": f(np.asarray(inp["d_skip"][0]).reshape(1, 32)),
        "ssm_norm_w": f(np.asarray(inp["ssm_norm_w"][0]).reshape(1, 2048)), "w_ssm_out": f(inp["w_ssm_out"][0]), "w_ret_out": f(inp["w_ret_out"][0]),
        "w_out": f(inp["w_out"][0]), "w_router": f(inp["w_router"][0]), "router_bias": f(np.asarray(inp["router_bias"][0]).reshape(1, 256)),
        "weL": weL,
        "w_sh_gate": f(inp["w_sh_gate"][0]), "w_sh_up": f(inp["w_sh_up"][0]), "w_sh_down": f(inp["w_sh_down"][0]),
        "final_norm_w": f(np.asarray(inp["final_norm_w"]).reshape(1, 1024)),
    }


def kernel(**inputs):
    B, S = inputs["x"].shape[0], inputs["x"].shape[1]
    nc = bass.Bass("TRN2", target_bir_lowering=False)
    L = build(nc, S)
    L["P"].emit(L["st"])
    L["st"].close()
    shared = _host_inputs(inputs, 0, S)
    in_maps = []
    for b in range(B):
        m = dict(shared)
        m["x"] = np.ascontiguousarray(np.asarray(inputs["x"][b], dtype=np.float32))
        m["pos"] = np.ascontiguousarray(np.asarray(inputs["positions"][b], dtype=np.int32).reshape(1, S))
        m["ccol"] = np.ascontiguousarray(np.asarray(inputs["c"][b], dtype=np.float32).reshape(8, 128).T)
        in_maps.append(m)
    res = run_bass_kernel_spmd(nc, in_maps, core_ids=list(range(B)))
    return np.stack([np.asarray(r["out"], dtype=np.float32) for r in res.results], axis=0)
```
